# Optimizing a Trainium2 kernel written in Bass

```python
import math
import jax, jax.numpy as jnp
from jax import lax
import numpy as np

D_MODEL = 2048
BATCH = 2
SEQ = 8192
DEPTH = 1

S5_WIDTH = 1024
S5_GROUP = 16
S5_GROUPS = S5_WIDTH // S5_GROUP
S5_STATE = 64
DT_MIN = 1e-3
DT_MAX = 1e-1
CONV_WIDTH = 1024
CONV_K = 31
IN_COLS = S5_WIDTH + 2 * CONV_WIDTH + 2 * D_MODEL
N_MEM = 256
XATTN_HEADS = 4
XATTN_HEAD_DIM = D_MODEL // XATTN_HEADS
MOE_GROUPS = 8
EXPERTS_PER_GROUP = 8
N_EXPERTS = MOE_GROUPS * EXPERTS_PER_GROUP
TOP_K_FINE = 2
D_EXPERT = 512
MOE_BLOCK = 128

RMS_EPS = 1e-6
LN_EPS = 1e-5

kernel_name = "hybrid_s5_conformer_xattn_hmoe"


def rms_norm(x, g):
    xf = x.astype(jnp.float32)
    y = xf * lax.rsqrt(jnp.mean(xf * xf, axis=-1, keepdims=True) + RMS_EPS)
    return (y * g.astype(jnp.float32)).astype(x.dtype)


def _cmul(ar, ai, br, bi):
    return ar * br - ai * bi, ar * bi + ai * br


def s5_mixer(u, lam_re, lam_im, log_dt, b_re, b_im, c_re, c_im, d_skip):
    f32 = jnp.float32
    bsz, L = u.shape[0], u.shape[1]
    dt = jnp.exp(log_dt.astype(f32))[:, None]
    lr, li = lam_re.astype(f32), lam_im.astype(f32)
    mag = jnp.exp(lr * dt)
    ang = li * dt
    lb_re, lb_im = mag * jnp.cos(ang), mag * jnp.sin(ang)
    den = lr * lr + li * li
    nr, ni = lb_re - 1.0, lb_im
    coef_re = (nr * lr + ni * li) / den
    coef_im = (ni * lr - nr * li) / den
    bb_re, bb_im = _cmul(coef_re[..., None], coef_im[..., None],
                         b_re.astype(f32), b_im.astype(f32))
    ug = u.astype(f32).reshape(bsz, L, S5_GROUPS, S5_GROUP)
    bu_re = jnp.einsum('blgc,gnc->blgn', ug, bb_re)
    bu_im = jnp.einsum('blgc,gnc->blgn', ug, bb_im)
    a_re = jnp.broadcast_to(lb_re, (1, L, S5_GROUPS, S5_STATE))
    a_im = jnp.broadcast_to(lb_im, (1, L, S5_GROUPS, S5_STATE))

    def combine(e1, e2):
        a1r, a1i, b1r, b1i = e1
        a2r, a2i, b2r, b2i = e2
        ar, ai = _cmul(a2r, a2i, a1r, a1i)
        br, bi = _cmul(a2r, a2i, b1r, b1i)
        return (ar, ai, br + b2r, bi + b2i)

    _, _, xr, xi = lax.associative_scan(combine, (a_re, a_im, bu_re, bu_im), axis=1)
    y = (jnp.einsum('blgn,gcn->blgc', xr, c_re.astype(f32))
         - jnp.einsum('blgn,gcn->blgc', xi, c_im.astype(f32))
         + d_skip.astype(f32).reshape(S5_GROUPS, S5_GROUP) * ug)
    return y.reshape(bsz, L, S5_WIDTH).astype(u.dtype)


def conformer_conv(v, dw_w, dw_b, ln_g, ln_b, w_out):
    a, b = jnp.split(v, 2, axis=-1)
    z = a * jax.nn.sigmoid(b)
    z = lax.conv_general_dilated(
        z, dw_w[:, None, :], window_strides=(1,), padding=[(CONV_K - 1, 0)],
        dimension_numbers=('NWC', 'WIO', 'NWC'),
        feature_group_count=CONV_WIDTH) + dw_b
    zf = z.astype(jnp.float32)
    mu = jnp.mean(zf, axis=-1, keepdims=True)
    var = jnp.mean(jnp.square(zf - mu), axis=-1, keepdims=True)
    zf = (zf - mu) * lax.rsqrt(var + LN_EPS) * ln_g.astype(jnp.float32) + ln_b.astype(jnp.float32)
    z = jax.nn.silu(zf).astype(v.dtype)
    return z @ w_out


def cross_attn(h, m, wq, wk, wv, wo):
    bsz, L, _ = h.shape
    q = (h @ wq).reshape(bsz, L, XATTN_HEADS, XATTN_HEAD_DIM)
    k = (m @ wk).reshape(bsz, N_MEM, XATTN_HEADS, XATTN_HEAD_DIM)
    v = (m @ wv).reshape(bsz, N_MEM, XATTN_HEADS, XATTN_HEAD_DIM)
    s = jnp.einsum('blhd,bmhd->bhlm', q, k).astype(jnp.float32) / math.sqrt(XATTN_HEAD_DIM)
    p = jax.nn.softmax(s, axis=-1).astype(h.dtype)
    o = jnp.einsum('bhlm,bmhd->blhd', p, v).reshape(bsz, L, D_MODEL)
    return o @ wo


def hier_moe(h, w_rg, b_rg, w_re, b_re, w_gate, w_up, w_down):
    bsz, L, D = h.shape
    T = bsz * L
    TK = T * TOP_K_FINE
    hf = h.reshape(T, D)
    cl = (hf @ w_rg).astype(jnp.float32) + b_rg.astype(jnp.float32)
    pg = jax.nn.softmax(cl, axis=-1)
    g_idx = jnp.argmax(cl, axis=-1).astype(jnp.int32)
    p_sel = jnp.take_along_axis(pg, g_idx[:, None], axis=1)[:, 0]
    fl = jnp.einsum('td,gde->tge', hf, w_re).astype(jnp.float32) + b_re.astype(jnp.float32)
    fl_sel = jnp.take_along_axis(fl, g_idx[:, None, None], axis=1)[:, 0]
    top_v, top_i = lax.top_k(fl_sel, TOP_K_FINE)
    w = p_sel[:, None] * jax.nn.softmax(top_v, axis=-1)
    e_ids = (g_idx[:, None] * EXPERTS_PER_GROUP + top_i).reshape(-1).astype(jnp.int32)
    w_flat = w.reshape(-1)
    tok = jnp.arange(TK, dtype=jnp.int32) // TOP_K_FINE

    order = jnp.argsort(e_ids)
    se = e_ids[order]
    counts = jax.ops.segment_sum(jnp.ones((TK,), jnp.int32), e_ids, num_segments=N_EXPERTS)
    starts = jnp.cumsum(counts) - counts
    pcounts = ((counts + MOE_BLOCK - 1) // MOE_BLOCK) * MOE_BLOCK
    pends = jnp.cumsum(pcounts)
    pstarts = pends - pcounts
    dest = pstarts[se] + jnp.arange(TK, dtype=jnp.int32) - starts[se]
    n_blocks = -(-TK // MOE_BLOCK) + N_EXPERTS
    n_rows = n_blocks * MOE_BLOCK
    row_tok = jnp.zeros((n_rows,), jnp.int32).at[dest].set(tok[order])
    row_w = jnp.zeros((n_rows,), jnp.float32).at[dest].set(w_flat[order])
    block_start = jnp.arange(n_blocks, dtype=jnp.int32) * MOE_BLOCK
    block_e = jnp.minimum(jnp.searchsorted(pends, block_start, side='right'),
                          N_EXPERTS - 1).astype(jnp.int32)
    xs = hf[row_tok].reshape(n_blocks, MOE_BLOCK, D)

    def expert_block(args):
        xb, e = args
        return (jax.nn.silu(xb @ w_gate[e]) * (xb @ w_up[e])) @ w_down[e]

    ys = lax.map(expert_block, (xs, block_e)).reshape(n_rows, D)
    ys = ys * row_w[:, None].astype(ys.dtype)
    out = jnp.zeros((T, D), h.dtype).at[row_tok].add(ys)
    return out.reshape(bsz, L, D)


def setup_inputs(seed: int = 0) -> dict:
    key = jax.random.key(seed)
    ks = jax.random.split(key, 32)
    f32 = jnp.float32

    def nrm(k, shape, scale):
        return jax.random.normal(k, shape, f32) * scale

    def gain(k, shape):
        return 1.0 + 0.02 * jax.random.normal(k, shape, f32)

    Dm = D_MODEL
    n_idx = jnp.arange(S5_STATE, dtype=f32)
    lam_re = -0.5 + 0.01 * jax.random.normal(ks[4], (DEPTH, S5_GROUPS, S5_STATE), f32)
    lam_im = math.pi * n_idx + 0.01 * jax.random.normal(ks[5], (DEPTH, S5_GROUPS, S5_STATE), f32)
    log_dt = jax.random.uniform(ks[6], (DEPTH, S5_GROUPS), f32,
                                math.log(DT_MIN), math.log(DT_MAX))
    return {
        "x": nrm(ks[0], (BATCH, SEQ, Dm), 1.0),
        "mem": nrm(ks[1], (BATCH, N_MEM, Dm), 1.0),
        "norm_mix_g": gain(ks[2], (DEPTH, Dm)),
        "w_in": nrm(ks[3], (DEPTH, Dm, IN_COLS), Dm ** -0.5),
        "s5_lambda_re": lam_re,
        "s5_lambda_im": lam_im,
        "s5_log_dt": log_dt,
        "s5_b_re": nrm(ks[7], (DEPTH, S5_GROUPS, S5_STATE, S5_GROUP), (2 * S5_GROUP) ** -0.5),
        "s5_b_im": nrm(ks[8], (DEPTH, S5_GROUPS, S5_STATE, S5_GROUP), (2 * S5_GROUP) ** -0.5),
        "s5_c_re": nrm(ks[9], (DEPTH, S5_GROUPS, S5_GROUP, S5_STATE), S5_STATE ** -0.5),
        "s5_c_im": nrm(ks[10], (DEPTH, S5_GROUPS, S5_GROUP, S5_STATE), S5_STATE ** -0.5),
        "s5_d": nrm(ks[11], (DEPTH, S5_WIDTH), 1.0),
        "s5_w_glu": nrm(ks[12], (DEPTH, S5_WIDTH, 2 * Dm), S5_WIDTH ** -0.5),
        "conv_dw_w": nrm(ks[13], (DEPTH, CONV_K, CONV_WIDTH), CONV_K ** -0.5),
        "conv_dw_b": nrm(ks[14], (DEPTH, CONV_WIDTH), 0.02),
        "conv_ln_g": gain(ks[15], (DEPTH, CONV_WIDTH)),
        "conv_ln_b": nrm(ks[16], (DEPTH, CONV_WIDTH), 0.02),
        "conv_w_out": nrm(ks[17], (DEPTH, CONV_WIDTH, Dm), CONV_WIDTH ** -0.5),
        "w_out": nrm(ks[18], (DEPTH, Dm, Dm), Dm ** -0.5),
        "norm_xattn_g": gain(ks[19], (DEPTH, Dm)),
        "norm_mem_g": gain(ks[20], (DEPTH, Dm)),
        "xattn_wq": nrm(ks[21], (DEPTH, Dm, Dm), Dm ** -0.5),
        "xattn_wk": nrm(ks[22], (DEPTH, Dm, Dm), Dm ** -0.5),
        "xattn_wv": nrm(ks[23], (DEPTH, Dm, Dm), Dm ** -0.5),
        "xattn_wo": nrm(ks[24], (DEPTH, Dm, Dm), Dm ** -0.5),
        "norm_moe_g": gain(ks[25], (DEPTH, Dm)),
        "router_w_group": nrm(ks[26], (DEPTH, Dm, MOE_GROUPS), Dm ** -0.5),
        "router_b_group": nrm(ks[27], (DEPTH, MOE_GROUPS), 0.01),
        "router_w_expert": nrm(ks[28], (DEPTH, MOE_GROUPS, Dm, EXPERTS_PER_GROUP), Dm ** -0.5),
        "router_b_expert": nrm(ks[29], (DEPTH, MOE_GROUPS, EXPERTS_PER_GROUP), 0.01),
        "exp_w_gate": nrm(ks[30], (DEPTH, N_EXPERTS, Dm, D_EXPERT), Dm ** -0.5),
        "exp_w_up": nrm(ks[31], (DEPTH, N_EXPERTS, Dm, D_EXPERT), Dm ** -0.5),
        "exp_w_down": nrm(jax.random.fold_in(key, 101), (DEPTH, N_EXPERTS, D_EXPERT, Dm), D_EXPERT ** -0.5),
        "norm_final_g": gain(jax.random.fold_in(key, 102), (Dm,)),
    }


def reference(x, mem, norm_mix_g, w_in, s5_lambda_re, s5_lambda_im, s5_log_dt,
              s5_b_re, s5_b_im, s5_c_re, s5_c_im, s5_d, s5_w_glu,
              conv_dw_w, conv_dw_b, conv_ln_g, conv_ln_b, conv_w_out, w_out,
              norm_xattn_g, norm_mem_g, xattn_wq, xattn_wk, xattn_wv, xattn_wo,
              norm_moe_g, router_w_group, router_b_group, router_w_expert, router_b_expert,
              exp_w_gate, exp_w_up, exp_w_down, norm_final_g):
    for l in range(DEPTH):
        h = rms_norm(x, norm_mix_g[l])
        proj = h @ w_in[l]
        u_s5 = proj[..., :S5_WIDTH]
        v_conv = proj[..., S5_WIDTH:S5_WIDTH + 2 * CONV_WIDTH]
        gates = jax.nn.sigmoid(proj[..., S5_WIDTH + 2 * CONV_WIDTH:])
        gate_a, gate_b = jnp.split(gates, 2, axis=-1)

        y_s5 = s5_mixer(u_s5, s5_lambda_re[l], s5_lambda_im[l], s5_log_dt[l],
                        s5_b_re[l], s5_b_im[l], s5_c_re[l], s5_c_im[l], s5_d[l])
        z = jax.nn.gelu(y_s5)
        val, gt = jnp.split(z @ s5_w_glu[l], 2, axis=-1)
        y_a = val * jax.nn.sigmoid(gt)

        y_b = conformer_conv(v_conv, conv_dw_w[l], conv_dw_b[l], conv_ln_g[l],
                             conv_ln_b[l], conv_w_out[l])

        x = x + (gate_a * y_a + gate_b * y_b) @ w_out[l]

        h = rms_norm(x, norm_xattn_g[l])
        m = rms_norm(mem, norm_mem_g[l])
        x = x + cross_attn(h, m, xattn_wq[l], xattn_wk[l], xattn_wv[l], xattn_wo[l])

        h = rms_norm(x, norm_moe_g[l])
        x = x + hier_moe(h, router_w_group[l], router_b_group[l], router_w_expert[l],
                         router_b_expert[l], exp_w_gate[l], exp_w_up[l], exp_w_down[l])
    return rms_norm(x, norm_final_g)
```

```python
import contextlib
import math
import numpy as np
import concourse.bass as bass
import concourse.mybir as mybir
from concourse.bass_utils import run_bass_kernel_spmd

F32 = mybir.dt.float32
BF16 = mybir.dt.bfloat16
I32 = mybir.dt.int32
AF = mybir.ActivationFunctionType
ALU = mybir.AluOpType
AX = mybir.AxisListType

NCORES = 8
D = 2048
TC = 2048
NT = TC // 128
HIST = 6144
NMEM = 256
NEXP = 64
DE = 512
QC = 8
NCH = TC // QC
RMS_EPS = 1e-6
LN_EPS = 1e-5
PI = math.pi


class Tracker:
    def __init__(self, nc, es):
        self.nc = nc
        self.es = es
        self.E = {}
        for name, obj in (("pe", nc.tensor), ("act", nc.scalar), ("dve", nc.vector),
                          ("pool", nc.gpsimd), ("sp", nc.sync)):
            sem = es.enter_context(nc.semaphore("sem_" + name))
            self.E[name] = dict(o=obj, sem=sem, n=0, seen={})
        self.wr = {}
        self.rd = {}
        self.dsem = {}

    def _wait(self, en, toks):
        e = self.E[en]
        for (k, sem, val) in toks:
            if e["seen"].get(k, 0) >= val:
                continue
            e["o"].wait_ge(sem, val)
            e["seen"][k] = val

    def _deps(self, reads, writes):
        toks = []
        for r in reads:
            if r in self.wr:
                toks.append(self.wr[r])
        for w in writes:
            if w in self.wr:
                toks.append(self.wr[w])
            toks.extend(self.rd.get(w, {}).values())
        return toks

    def _commit(self, tok, reads, writes):
        for r in reads:
            d = self.rd.setdefault(r, {})
            if tok[0] not in d or d[tok[0]][2] < tok[2]:
                d[tok[0]] = tok
        for w in writes:
            self.wr[w] = tok
            self.rd[w] = {}

    def I(self, en, reads, writes, fn):
        self._wait(en, self._deps(reads, writes))
        e = self.E[en]
        ins = fn(e["o"])
        e["n"] += 1
        ins.then_inc(e["sem"], 1)
        tok = (en, e["sem"], e["n"])
        self._commit(tok, reads, writes)
        return tok

    def MM(self, reads, writes, fns):
        self._wait("pe", self._deps(reads, writes))
        e = self.E["pe"]
        ins = None
        for f in fns:
            ins = f(e["o"])
        e["n"] += 1
        ins.then_inc(e["sem"], 1)
        tok = ("pe", e["sem"], e["n"])
        self._commit(tok, reads, writes)
        return tok

    def DMA(self, en, dname, reads, writes, fn):
        if dname not in self.dsem:
            self.dsem[dname] = [self.es.enter_context(self.nc.semaphore("d_" + dname)), 0]
        self._wait(en, self._deps(reads, writes))
        ds = self.dsem[dname]
        ins = fn(self.E[en]["o"])
        ds[1] += 16
        ins.then_inc(ds[0], 16)
        tok = ("d_" + dname, ds[0], ds[1])
        self._commit(tok, reads, writes)
        return tok

    def barrier(self):
        toks = [(n, e["sem"], e["n"]) for n, e in self.E.items() if e["n"] > 0]
        toks += [("d_" + k, v[0], v[1]) for k, v in self.dsem.items() if v[1] > 0]
        for en in self.E:
            self._wait(en, toks)
        self.wr = {}
        self.rd = {}


def build_nc(stop=None):
    nc = bass.Bass("TRN2", target_bir_lowering=False)

    def din(name, shape, dt=F32):
        return nc.dram_tensor(name, list(shape), dt, kind="ExternalInput").ap()

    xm = din("xm", [TC, D])
    xh = din("xh", [HIST, D])
    memb = din("memb", [NMEM, D])
    w_in = din("w_in", [D, 7168])
    w_glu = din("w_glu", [1024, 4096])
    w_cwo = din("w_cwo", [1024, D])
    w_out = din("w_out", [D, D])
    w_q = din("w_q", [D, D])
    w_k = din("w_k", [D, D])
    w_v = din("w_v", [D, D])
    w_o = din("w_o", [D, D])
    gains = din("gains", [5, 128, D])
    lam3 = din("lam3", [128, 3, 32])
    bc4 = din("bc4", [128, 4, 32, 32])
    chp = din("chp", [128, 8, 36])
    wr_d = din("wr", [D, 72])
    brep = din("brep", [128, 72])
    if stop is None:
        e_g = din("e_g", [NEXP, D, DE])
        e_u = din("e_u", [NEXP, D, DE])
        e_d = din("e_d", [NEXP, DE, D])
    cst = din("cst", [128, 6, 128])
    out_d = nc.dram_tensor("out", [TC, D], F32, kind="ExternalOutput").ap()

    def dscr(name, shape, dt):
        return nc.dram_tensor(name, list(shape), dt, kind="Internal").ap()

    m_d = dscr("m_d", [D, TC], BF16)
    x1_d = dscr("x1_d", [TC, D], F32)
    x2_d = dscr("x2_d", [TC, D], F32)
    h3_d = dscr("h3_d", [TC, D], BF16)
    y_d = dscr("y_d", [NEXP * 128 + 128, D], F32)
    if stop is None:
        eg_bf = dscr("eg_bf", [NEXP, 128, 16 * DE], BF16)
        tbl_d = dscr("tbl_d", [NEXP * 128, 4], F32)
        eu_bf = dscr("eu_bf", [NEXP, 128, 16 * DE], BF16)
        ed_bf = dscr("ed_bf", [NEXP, 128, 4 * D], BF16)

    dbg = {}

    with contextlib.ExitStack() as es:
        T = Tracker(nc, es)

        def sb(st, name, shape, dt):
            return st.enter_context(nc.sbuf_tensor(name, list(shape), dt))

        def pst(st, name, shape, dt):
            return st.enter_context(nc.psum_tensor(name, list(shape), dt))

        cst_t = sb(es, "cst_t", [128, 6, 128], F32)
        identb = sb(es, "identb", [128, 128], BF16)
        trib = sb(es, "trib", [128, 128], BF16)
        onesb = sb(es, "onesb", [128, 128], BF16)
        gb = sb(es, "gb", [128, D], F32)
        chp_t = sb(es, "chp_t", [128, 8, 36], F32)
        stat = sb(es, "stat", [128, 8], F32)
        epsc = sb(es, "epsc", [128, 2], F32)
        LamA = sb(es, "LamA", [128, 32, 2], F32)
        LamB = sb(es, "LamB", [128, 32, 2], F32)
        LamA64 = sb(es, "LamA64", [128, 32, 2], F32)
        LamB64 = sb(es, "LamB64", [128, 32, 2], F32)
        Xseg = sb(es, "Xseg", [128, 32, 2, 4], F32)
        sctS = sb(es, "sctS", [128, 2, 32, 2, 4], F32)
        PW = sb(es, "PW", [128, 9, 2, 32], F32)
        Xst = sb(es, "Xst", [128, 32, 2], F32)
        sct = sb(es, "sct", [128, 2, 32, 2], F32)
        slots_i = sb(es, "slots_i", [128, NT, 2], I32)
        pu = contextlib.ExitStack()
        Bb = sb(pu, "Bb", [128, 2, 32, 32], F32)
        u_t = sb(pu, "u_t", [128, 8, TC], BF16)
        S_all = sb(pu, "S_all", [128, 32, 2, NCH], BF16)

        pb = [pst(es, "pb%d" % i, [128, 512], F32) for i in range(6)]
        tp = [pst(es, "tp%d" % i, [128, 1024], BF16) for i in range(2)]

        ident_f = cst_t[:, 0, :]
        iota_row = cst_t[:, 2, :]
        tokid = cst_t[:, 3, :]
        bmask = cst_t[:, 4, :]
        xcol = cst_t[:, 5, 0:64]

        T.DMA("sp", "c0", [], ["cst"], lambda e: e.dma_start(out=cst_t[:], in_=cst[:, :, :]))
        T.DMA("sp", "c0", [], ["chp"], lambda e: e.dma_start(out=chp_t[:], in_=chp[:, :, :]))
        T.I("dve", ["cst"], ["identb"], lambda e: e.tensor_copy(out=identb[:], in_=ident_f))
        T.I("dve", ["cst"], ["trib"], lambda e: e.tensor_copy(out=trib[:], in_=cst_t[:, 1, :]))
        T.I("dve", [], ["onesb"], lambda e: e.memset(onesb[:], 1.0))
        T.I("dve", [], ["epsc"], lambda e: e.memset(epsc[:, 0:1], RMS_EPS))
        T.I("dve", [], ["epsc"], lambda e: e.memset(epsc[:, 1:2], LN_EPS))

        def load_gain(i):
            T.DMA("sp", "gain", [], ["gb"], lambda e: e.dma_start(out=gb[:], in_=gains[i, :, :]))

        def rms_tile(xt, xres, outt, ores, junk, jres, gres="gb"):
            T.I("act", [xres], [jres, "ssq"],
                lambda e: e.activation(out=junk, in_=xt, func=AF.Square, accum_out=stat[:, 0:1]))
            T.I("act", ["ssq"], ["rstd"],
                lambda e: e.activation(out=stat[:, 1:2], in_=stat[:, 0:1], func=AF.Sqrt, scale=1.0 / D,
                                       bias=epsc[:, 0:1]))
            T.I("dve", ["rstd"], ["rstd"], lambda e: e.reciprocal(out=stat[:, 1:2], in_=stat[:, 1:2]))
            T.I("dve", [xres, "rstd", gres], [ores],
                lambda e: e.scalar_tensor_tensor(out=outt, in0=xt, scalar=stat[:, 1:2], in1=gb[:],
                                                 op0=ALU.mult, op1=ALU.mult))

        def transpose_to_fm(src, sres, dst_fn, dres, ncols=128, col0=0):
            for half in range(2):
                fns = []
                for j in range(8):
                    kc = half * 8 + j
                    fns.append(lambda e, j=j, kc=kc: e.transpose(
                        out=tp[half][:, j * 128:(j + 1) * 128], in_=src[:, kc * 128:(kc + 1) * 128],
                        identity=identb[:]))
                T.MM([sres, "identb"], ["tp%d" % half], fns)
                view = tp[half][:].rearrange("p (j c) -> p j c", c=128)[:, :, col0:col0 + ncols]
                eng = "act" if half == 0 else "dve"
                if eng == "act":
                    T.I("act", ["tp%d" % half], [dres],
                        lambda e, view=view, half=half: e.copy(out=dst_fn(half), in_=view))
                else:
                    T.I("dve", ["tp%d" % half], [dres],
                        lambda e, view=view, half=half: e.tensor_copy(out=dst_fn(half), in_=view))

        bg_state = [0]

        def bg(n):
            if stop is not None:
                return
            for _ in range(n):
                i = bg_state[0]
                if i >= 3 * NEXP:
                    return
                bg_state[0] += 1
                x, which = i // 3, i % 3
                if which == 0:
                    src, dst, kcn = e_g[x, :, :], eg_bf[x, :, :], 16
                elif which == 1:
                    src, dst, kcn = e_u[x, :, :], eu_bf[x, :, :], 16
                else:
                    src, dst, kcn = e_d[x, :, :], ed_bf[x, :, :], 4
                T.DMA("pool", "bg", [], ["ebf%d" % i],
                      lambda e, src=src, dst=dst, kcn=kcn: e.dma_start(
                          out=dst.rearrange("p (kc n) -> p kc n", kc=kcn),
                          in_=src.rearrange("(kc p) n -> p kc n", p=128)))

        def load_slab(wbuf, wres, dname, src_ap, ncolsK):
            T.DMA("pool", dname, [], [wres],
                  lambda e: e.dma_start(out=wbuf, in_=src_ap.rearrange("(kc p) n -> p kc n", p=128)))

        ph = contextlib.ExitStack()
        sm = sb(ph, "sm", [128, 20, 32], F32)
        bc_t = sb(ph, "bc_t", [128, 4, 32, 32], F32)
        smi = sb(ph, "smi", [128, 32], I32)
        T.DMA("sp", "c0", [], ["sm"], lambda e: e.dma_start(out=sm[:, 0:3, :], in_=lam3[:, :, :]))
        T.DMA("sp", "c0", [], ["bc"], lambda e: e.dma_start(out=bc_t[:], in_=bc4[:, :, :, :]))
        S = lambda i: sm[:, i, :]
        LRE, LIM, LDT, DT, MAG, ANG, TMP, SN, CS, LBRE, LBIM, DEN, NR, CR, CI, T1, T2 = range(17)

        def dv(fn):
            T.I("dve", ["sm", "bc", "PW", "Bb"], ["sm"], fn)

        def ac(fn):
            T.I("act", ["sm"], ["sm"], fn)

        ac(lambda e: e.activation(out=S(DT), in_=S(LDT), func=AF.Exp))
        dv(lambda e: e.tensor_tensor(out=S(TMP), in0=S(LRE), in1=S(DT), op=ALU.mult))
        ac(lambda e: e.activation(out=S(MAG), in_=S(TMP), func=AF.Exp))
        dv(lambda e: e.tensor_tensor(out=S(ANG), in0=S(LIM), in1=S(DT), op=ALU.mult))
        def sin_of(dst, phase):
            dv(lambda e: e.tensor_scalar(out=S(T1), in0=S(ANG), scalar1=1.0 / (2.0 * PI), scalar2=8.5 + phase,
                                         op0=ALU.mult, op1=ALU.add))
            dv(lambda e: e.tensor_copy(out=smi[:], in_=S(T1)))
            dv(lambda e: e.tensor_copy(out=S(T2), in_=smi[:]))
            dv(lambda e: e.tensor_tensor(out=S(T1), in0=S(T1), in1=S(T2), op=ALU.subtract))
            dv(lambda e: e.scalar_tensor_tensor(out=S(T1), in0=S(T1), scalar=0.0, in1=S(T1),
                                                op0=ALU.is_lt, op1=ALU.add))
            dv(lambda e: e.tensor_scalar(out=S(T1), in0=S(T1), scalar1=2.0 * PI, scalar2=-PI,
                                         op0=ALU.mult, op1=ALU.add))
            ac(lambda e: e.activation(out=S(dst), in_=S(T1), func=AF.Sin))

        sin_of(SN, 0.0)
        sin_of(CS, 0.25)
        dv(lambda e: e.tensor_tensor(out=S(LBRE), in0=S(MAG), in1=S(CS), op=ALU.mult))
        dv(lambda e: e.tensor_tensor(out=S(LBIM), in0=S(MAG), in1=S(SN), op=ALU.mult))
        dv(lambda e: e.tensor_tensor(out=S(T1), in0=S(LRE), in1=S(LRE), op=ALU.mult))
        dv(lambda e: e.tensor_tensor(out=S(T2), in0=S(LIM), in1=S(LIM), op=ALU.mult))
        dv(lambda e: e.tensor_tensor(out=S(DEN), in0=S(T1), in1=S(T2), op=ALU.add))
        dv(lambda e: e.reciprocal(out=S(DEN), in_=S(DEN)))
        dv(lambda e: e.tensor_scalar(out=S(NR), in0=S(LBRE), scalar1=-1.0, scalar2=None, op0=ALU.add))
        dv(lambda e: e.tensor_tensor(out=S(T1), in0=S(NR), in1=S(LRE), op=ALU.mult))
        dv(lambda e: e.tensor_tensor(out=S(T2), in0=S(LBIM), in1=S(LIM), op=ALU.mult))
        dv(lambda e: e.tensor_tensor(out=S(T1), in0=S(T1), in1=S(T2), op=ALU.add))
        dv(lambda e: e.tensor_tensor(out=S(CR), in0=S(T1), in1=S(DEN), op=ALU.mult))
        dv(lambda e: e.tensor_tensor(out=S(T1), in0=S(LBIM), in1=S(LRE), op=ALU.mult))
        dv(lambda e: e.tensor_tensor(out=S(T2), in0=S(NR), in1=S(LIM), op=ALU.mult))
        dv(lambda e: e.tensor_tensor(out=S(T1), in0=S(T1), in1=S(T2), op=ALU.subtract))
        dv(lambda e: e.tensor_tensor(out=S(CI), in0=S(T1), in1=S(DEN), op=ALU.mult))

        def pwd(fn):
            T.I("dve", ["sm", "PW"], ["PW", "sm"], fn)

        pwd(lambda e: e.memset(PW[:, 0, 0, :], 1.0))
        pwd(lambda e: e.memset(PW[:, 0, 1, :], 0.0))
        for k in range(1, 9):
            pr_, pi_ = PW[:, k - 1, 0, :], PW[:, k - 1, 1, :]
            pwd(lambda e, pr_=pr_: e.tensor_tensor(out=S(T1), in0=pr_, in1=S(LBRE), op=ALU.mult))
            pwd(lambda e, pi_=pi_: e.tensor_tensor(out=S(T2), in0=pi_, in1=S(LBIM), op=ALU.mult))
            pwd(lambda e, k=k: e.tensor_tensor(out=PW[:, k, 0, :], in0=S(T1), in1=S(T2), op=ALU.subtract))
            pwd(lambda e, pr_=pr_: e.tensor_tensor(out=S(T1), in0=pr_, in1=S(LBIM), op=ALU.mult))
            pwd(lambda e, pi_=pi_: e.tensor_tensor(out=S(T2), in0=pi_, in1=S(LBRE), op=ALU.mult))
            pwd(lambda e, k=k: e.tensor_tensor(out=PW[:, k, 1, :], in0=S(T1), in1=S(T2), op=ALU.add))
        pwd(lambda e: e.tensor_copy(out=LamA[:, :, 0], in_=PW[:, 8, 0, :]))
        pwd(lambda e: e.tensor_copy(out=LamA[:, :, 1], in_=PW[:, 8, 0, :]))
        pwd(lambda e: e.tensor_scalar(out=LamB[:, :, 0], in0=PW[:, 8, 1, :], scalar1=-1.0, scalar2=None,
                                      op0=ALU.mult))
        pwd(lambda e: e.tensor_copy(out=LamB[:, :, 1], in_=PW[:, 8, 1, :]))
        pwd(lambda e: e.tensor_copy(out=S(17), in_=PW[:, 8, 0, :]))
        pwd(lambda e: e.tensor_copy(out=S(18), in_=PW[:, 8, 1, :]))
        for _sq in range(6):
            pwd(lambda e: e.tensor_tensor(out=S(T1), in0=S(17), in1=S(17), op=ALU.mult))
            pwd(lambda e: e.tensor_tensor(out=S(T2), in0=S(18), in1=S(18), op=ALU.mult))
            pwd(lambda e: e.tensor_tensor(out=S(T1), in0=S(T1), in1=S(T2), op=ALU.subtract))
            pwd(lambda e: e.tensor_tensor(out=S(T2), in0=S(17), in1=S(18), op=ALU.mult))
            pwd(lambda e: e.tensor_scalar(out=S(18), in0=S(T2), scalar1=2.0, scalar2=None, op0=ALU.mult))
            pwd(lambda e: e.tensor_copy(out=S(17), in_=S(T1)))
        pwd(lambda e: e.tensor_copy(out=LamA64[:, :, 0], in_=S(17)))
        pwd(lambda e: e.tensor_copy(out=LamA64[:, :, 1], in_=S(17)))
        pwd(lambda e: e.tensor_scalar(out=LamB64[:, :, 0], in0=S(18), scalar1=-1.0, scalar2=None, op0=ALU.mult))
        pwd(lambda e: e.tensor_copy(out=LamB64[:, :, 1], in_=S(18)))

        def bcast(ap32):
            return ap32.unsqueeze(2).to_broadcast([128, 32, 32])

        with contextlib.ExitStack() as t0:
            tA = sb(t0, "tA", [128, 32, 32], F32)
            tB = sb(t0, "tB", [128, 32, 32], F32)

            def bd(fn):
                T.I("dve", ["sm", "bc", "tA"], ["Bb", "tA"], fn)

            bd(lambda e: e.tensor_tensor(out=tA[:], in0=bc_t[:, 0], in1=bcast(S(CR)), op=ALU.mult))
            bd(lambda e: e.tensor_tensor(out=tB[:], in0=bc_t[:, 1], in1=bcast(S(CI)), op=ALU.mult))
            bd(lambda e: e.tensor_tensor(out=Bb[:, 0], in0=tA[:], in1=tB[:], op=ALU.subtract))
            bd(lambda e: e.tensor_tensor(out=tA[:], in0=bc_t[:, 1], in1=bcast(S(CR)), op=ALU.mult))
            bd(lambda e: e.tensor_tensor(out=tB[:], in0=bc_t[:, 0], in1=bcast(S(CI)), op=ALU.mult))
            bd(lambda e: e.tensor_tensor(out=Bb[:, 1], in0=tA[:], in1=tB[:], op=ALU.add))
            T.barrier()
        ph.close()

        def make_pk(k, Pf, Pb_, tA, tB):
            def f(fn):
                T.I("dve", ["Bb", "PW", "pk"], ["pk"], fn)
            pr_, pi_ = bcast(PW[:, k, 0, :]), bcast(PW[:, k, 1, :])
            f(lambda e: e.tensor_tensor(out=tA[:], in0=Bb[:, 0], in1=pr_, op=ALU.mult))
            f(lambda e: e.tensor_tensor(out=tB[:], in0=Bb[:, 1], in1=pi_, op=ALU.mult))
            f(lambda e: e.tensor_tensor(out=Pb_[:, 0], in0=tA[:], in1=tB[:], op=ALU.subtract))
            f(lambda e: e.tensor_tensor(out=tA[:], in0=Bb[:, 0], in1=pi_, op=ALU.mult))
            f(lambda e: e.tensor_tensor(out=tB[:], in0=Bb[:, 1], in1=pr_, op=ALU.mult))
            f(lambda e: e.tensor_tensor(out=Pb_[:, 1], in0=tA[:], in1=tB[:], op=ALU.add))

        pa = contextlib.ExitStack()
        Wu = sb(pa, "Wu", [128, 16, 1024], BF16)
        W2 = sb(pa, "W2", [128, 8, 8, 2, 128], BF16)

        load_slab(Wu[:, :, 0:512], "Wu", "wu0", w_in[:, 0:512], 16)
        load_slab(Wu[:, :, 512:1024], "Wu", "wu1", w_in[:, 512:1024], 16)
        load_gain(0)

        with contextlib.ExitStack() as t0:
            Pb_ = sb(t0, "Pb_", [128, 2, 32, 32], BF16)
            tA = sb(t0, "tA2", [128, 32, 32], F32)
            tB = sb(t0, "tB2", [128, 32, 32], F32)
            for k in range(8):
                make_pk(k, None, Pb_, tA, tB)
                for ri in range(2):
                    fns = []
                    for pc in range(8):
                        fns.append(lambda e, pc=pc, ri=ri: e.transpose(
                            out=tp[ri][:, pc * 128:(pc + 1) * 128],
                            in_=Pb_[:, ri, pc * 4:(pc + 1) * 4, :].rearrange("p a b -> p (a b)"),
                            identity=identb[:]))
                    T.MM(["pk", "identb"], ["tp%d" % ri], fns)
                    T.I("act", ["tp%d" % ri], ["W2"],
                        lambda e, ri=ri, k=k: e.copy(out=W2[:, :, 7 - k, ri, :],
                                                     in_=tp[ri][:].rearrange("p (a b) -> p a b", b=128)))
            T.barrier()

        xring = [sb(pa, "xring%d" % i, [128, D], F32) for i in range(2)]
        xsb = [sb(pa, "xsb%d" % i, [128, D], BF16) for i in range(2)]
        junk = sb(pa, "junk", [128, D], BF16)
        h1blks = [sb(pa, "h1blk0", [128, 16, 512], BF16)]
        T.I("dve", [], ["Xst"], lambda e: e.memset(Xst[:], 0.0))
        Xv = Xst[:]
        Xsw = bass.AP(Xv.tensor, Xv.offset + 1, [list(Xv.ap[0]), [2, 32], [-1, 2]])

        def scan_step(c, store=None, eng="dve"):
            Sc = S_all[:, :, :, c]
            T.I(eng, ["Xst"], ["sc0"], lambda e: e.tensor_tensor(out=sct[:, 0], in0=LamA[:], in1=Xst[:], op=ALU.mult))
            T.I(eng, ["Xst"], ["sc1"], lambda e: e.tensor_tensor(out=sct[:, 1], in0=LamB[:], in1=Xsw, op=ALU.mult))
            T.I(eng, ["sc0", "sc1"], ["sc0"], lambda e: e.tensor_tensor(out=sct[:, 0], in0=sct[:, 0], in1=sct[:, 1], op=ALU.add))
            T.I(eng, ["sc0", "S_all"], ["Xst"], lambda e: e.tensor_tensor(out=Xst[:], in0=sct[:, 0], in1=Sc, op=ALU.add))
            if store is not None:
                T.I("dve", ["Xst"], ["X_all"], lambda e: e.tensor_copy(out=store, in_=Xst[:]))

        Xg = Xseg[:]
        Xseg_sw = bass.AP(Xg.tensor, Xg.offset + 4, [list(Xg.ap[0]), [8, 32], [-4, 2], [1, 4]])
        LamA_b = LamA[:].unsqueeze(3).to_broadcast([128, 32, 2, 4])
        LamB_b = LamB[:].unsqueeze(3).to_broadcast([128, 32, 2, 4])

        def hist_scan():
            eng = "pool"
            T.I(eng, ["Xseg"], ["Xseg"], lambda e: e.memset(Xseg[:], 0.0))
            for c in range(64):
                Sv = S_all[:, :, :, c::64]
                T.I(eng, ["Xseg"], ["ss0"], lambda e: e.tensor_tensor(out=sctS[:, 0], in0=LamA_b, in1=Xseg[:], op=ALU.mult))
                T.I(eng, ["Xseg"], ["ss1"], lambda e: e.tensor_tensor(out=sctS[:, 1], in0=LamB_b, in1=Xseg_sw, op=ALU.mult))
                T.I(eng, ["ss0", "ss1"], ["ss0"], lambda e: e.tensor_tensor(out=sctS[:, 0], in0=sctS[:, 0], in1=sctS[:, 1], op=ALU.add))
                T.I(eng, ["ss0", "S_all"], ["Xseg"], lambda e, Sv=Sv: e.tensor_tensor(out=Xseg[:], in0=sctS[:, 0], in1=Sv, op=ALU.add))
            for sg_ in range(4):
                T.I(eng, ["Xst"], ["sc0"], lambda e: e.tensor_tensor(out=sct[:, 0], in0=LamA64[:], in1=Xst[:], op=ALU.mult))
                T.I(eng, ["Xst"], ["sc1"], lambda e: e.tensor_tensor(out=sct[:, 1], in0=LamB64[:], in1=Xsw, op=ALU.mult))
                T.I(eng, ["sc0", "sc1"], ["sc0"], lambda e: e.tensor_tensor(out=sct[:, 0], in0=sct[:, 0], in1=sct[:, 1], op=ALU.add))
                T.I(eng, ["sc0", "Xseg"], ["Xst"], lambda e, sg_=sg_: e.tensor_tensor(out=Xst[:], in0=sct[:, 0], in1=Xseg[:, :, :, sg_], op=ALU.add))

        tile_ctr = [0]
        blk_ctr = [0]
        for sbk in range(4):
            for blk in range(4):
                h1blk = h1blks[0]
                h1res = "h1blk0"
                blk_ctr[0] += 1
                for t in range(4):
                    ti = blk * 4 + t
                    r = tile_ctr[0] % 2
                    tile_ctr[0] += 1
                    src = (xh[sbk * TC + ti * 128: sbk * TC + (ti + 1) * 128, :] if sbk < 3
                           else xm[ti * 128:(ti + 1) * 128, :])
                    T.DMA("sp", "xr%d" % r, [], ["xr%d" % r], lambda e, r=r, src=src: e.dma_start(out=xring[r][:], in_=src))
                    rms_tile(xring[r][:], "xr%d" % r, xsb[r][:], "xs%d" % r, junk[:], "junk")
                    transpose_to_fm(xsb[r], "xs%d" % r,
                                    lambda half, t=t, h1blk=h1blk: h1blk[:, half * 8:(half + 1) * 8, t * 128:(t + 1) * 128],
                                    h1res)
                bg(3)
                for pc in range(8):
                    bank = pc % 2
                    fns = [lambda e, kc=kc, pc=pc, bank=bank, h1blk=h1blk: e.matmul(
                        out=pb[bank][:, :], lhsT=Wu[:, kc, pc * 128:(pc + 1) * 128], rhs=h1blk[:, kc, :],
                        start=(kc == 0), stop=(kc == 15)) for kc in range(16)]
                    T.MM([h1res, "Wu"], ["pb%d" % bank], fns)
                    T.I("act", ["pb%d" % bank], ["u%d_%d" % (pc, blk)],
                        lambda e, pc=pc, blk=blk, bank=bank: e.copy(out=u_t[:, pc, blk * 512:(blk + 1) * 512], in_=pb[bank][:, :]))
            for prr in range(32):
                pc, a = prr // 4, prr % 4
                bank = 2 + prr % 2
                fns = []
                for ri in range(2):
                    for i in range(8):
                        fns.append(lambda e, ri=ri, i=i, pc=pc, a=a, bank=bank: e.matmul(
                            out=pb[bank][:, ri * NCH:(ri + 1) * NCH],
                            lhsT=W2[32 * a:32 * a + 32, pc, i, ri, :],
                            rhs=u_t[32 * a:32 * a + 32, pc, i::QC],
                            start=(i == 0), stop=(i == 7), tile_position=(32 * a, 0)))
                T.MM(["u%d_%d" % (pc, b) for b in range(4)] + ["W2"], ["pb%d" % bank], fns)
                T.I("act", ["pb%d" % bank], ["S_all"],
                    lambda e, prr=prr, bank=bank: e.copy(out=S_all[:, prr, :, :],
                                                         in_=pb[bank][:, :].rearrange("p (r c) -> p r c", r=2)))
            if sbk < 3:
                hist_scan()
        T.barrier()
        pa.close()

        if stop == "A":
            dbg["u"] = (u_t, [128, 8, TC], BF16)
            dbg["S_all"] = (S_all, [128, 32, 2, NCH], BF16)
            dbg["Xst"] = (Xst, [128, 32, 2], F32)

        if stop not in ("A",):
            pbx = contextlib.ExitStack()
            Kmat = sb(pbx, "Kmat", [128, 8, 8, 128], BF16)
            G = sb(pbx, "G", [128, 32, 8, 2, 32], BF16)
            X_all = sb(pbx, "X_all", [128, 32, 2, NCH], BF16)
            bc_t2 = sb(pbx, "bc_t2", [128, 2, 32, 32], F32)
            T.DMA("sp", "c0", [], ["bc2"], lambda e: e.dma_start(out=bc_t2[:], in_=bc4[:, 2:4, :, :]))
            with contextlib.ExitStack() as t0:
                Pb_ = sb(t0, "Pb2_", [128, 2, 32, 32], BF16)
                Cb = sb(t0, "Cb", [128, 2, 32, 32], BF16)
                tA = sb(t0, "tA3", [128, 32, 32], F32)
                tB = sb(t0, "tB3", [128, 32, 32], F32)
                T.I("dve", ["bc2"], ["Cb"], lambda e: e.tensor_copy(out=Cb[:, 0], in_=bc_t2[:, 0]))
                T.I("dve", ["bc2"], ["Cb"], lambda e: e.tensor_scalar(out=Cb[:, 1], in0=bc_t2[:, 1], scalar1=-1.0,
                                                                      scalar2=None, op0=ALU.mult))
                for k in range(8):
                    make_pk(k, None, Pb_, tA, tB)
                    for pc in range(8):
                        bank = pc % 2
                        fns = [lambda e, ri=ri, pc=pc, bank=bank: e.matmul(
                            out=pb[bank][:, 0:128],
                            lhsT=Pb_[:, ri, pc * 4:(pc + 1) * 4, :].rearrange("p a b -> p (a b)"),
                            rhs=Cb[:, ri, pc * 4:(pc + 1) * 4, :].rearrange("p a b -> p (a b)"),
                            start=(ri == 0), stop=(ri == 1)) for ri in range(2)]
                        T.MM(["pk", "Cb"], ["pb%d" % bank], fns)
                        T.I("dve", ["pb%d" % bank, "cst"], ["Kmat"],
                            lambda e, pc=pc, k=k, bank=bank: e.tensor_tensor(
                                out=Kmat[:, pc, k, :], in0=pb[bank][:, 0:128], in1=bmask, op=ALU.mult))
                for pc in range(8):
                    T.I("dve", ["Kmat", "chp", "cst"], ["Kmat"],
                        lambda e, pc=pc: e.scalar_tensor_tensor(out=Kmat[:, pc, 0, :], in0=ident_f,
                                                                scalar=chp_t[:, pc, 0:1], in1=Kmat[:, pc, 0, :],
                                                                op0=ALU.mult, op1=ALU.add))
                for j in range(8):
                    pr_, pi_ = bcast(PW[:, j + 1, 0, :]), bcast(PW[:, j + 1, 1, :])

                    def g(fn):
                        T.I("dve", ["bc2", "PW", "gt"], ["G", "gt"], fn)
                    g(lambda e: e.tensor_tensor(out=tA[:], in0=bc_t2[:, 0], in1=pr_, op=ALU.mult))
                    g(lambda e: e.tensor_tensor(out=tB[:], in0=bc_t2[:, 1], in1=pi_, op=ALU.mult))
                    g(lambda e, j=j: e.tensor_tensor(out=G[:, :, j, 0, :], in0=tA[:], in1=tB[:], op=ALU.subtract))
                    g(lambda e: e.tensor_tensor(out=tA[:], in0=bc_t2[:, 0], in1=pi_, op=ALU.mult))
                    g(lambda e: e.tensor_tensor(out=tB[:], in0=bc_t2[:, 1], in1=pr_, op=ALU.mult))
                    g(lambda e: e.tensor_tensor(out=tA[:], in0=tA[:], in1=tB[:], op=ALU.add))
                    g(lambda e, j=j: e.tensor_scalar(out=G[:, :, j, 1, :], in0=tA[:], scalar1=-1.0, scalar2=None,
                                                     op0=ALU.mult))
                T.barrier()
            T.I("dve", ["Xst"], ["X_all"], lambda e: e.tensor_copy(out=X_all[:, :, :, 0], in_=Xst[:]))
            for c in range(NCH - 1):
                scan_step(c, store=X_all[:, :, :, c + 1])
            for pc in range(8):
                bg(3)
                for b in range(4):
                    bank = (pc * 4 + b) % 4
                    fns = []
                    for j in range(8):
                        for i in range(j + 1):
                            fns.append(lambda e, j=j, i=i, pc=pc, b=b, bank=bank: e.matmul(
                                out=pb[bank][:, j::QC], lhsT=Kmat[:, pc, j - i, :],
                                rhs=u_t[:, pc, b * 512 + i:(b + 1) * 512:QC],
                                start=(i == 0 and j == 0), stop=False))
                    for a in range(4):
                        prr = pc * 4 + a
                        for ri in range(2):
                            for j in range(8):
                                fns.append(lambda e, j=j, a=a, ri=ri, prr=prr, b=b, bank=bank: e.matmul(
                                    out=pb[bank][32 * a:32 * a + 32, j::QC], lhsT=G[:, prr, j, ri, :],
                                    rhs=X_all[:, prr, ri, b * 64:(b + 1) * 64],
                                    start=False, stop=(ri == 1 and j == 7), tile_position=(0, 32 * a)))
                    T.MM(["u%d_%d" % (pc, b), "Kmat", "G", "X_all"], ["pb%d" % bank], fns)
                    T.I("act", ["pb%d" % bank], ["u%d_%d" % (pc, b)],
                        lambda e, pc=pc, b=b, bank=bank: e.activation(
                            out=u_t[:, pc, b * 512:(b + 1) * 512], in_=pb[bank][:, :], func=AF.Gelu_apprx_tanh))
            T.barrier()
            if stop == "B":
                for name, tile, shape, dt in (("Kmat", Kmat, [128, 8, 8, 128], BF16), ("G", G, [128, 32, 8, 2, 32], BF16),
                                              ("X_all", X_all, [128, 32, 2, NCH], BF16)):
                    dd = nc.dram_tensor("dbg_" + name, list(shape), dt, kind="ExternalOutput").ap()
                    T.DMA("sp", "dbg", [], [], lambda e, dd=dd, tile=tile: e.dma_start(out=dd, in_=tile[:]))
                T.barrier()
            pbx.close()
            if stop == "B":
                dbg["zg"] = (u_t, [128, 8, TC], BF16)


        class Ring:
            def __init__(self, st, name, n, shape, dt):
                self.name = name
                self.tiles = [sb(st, "%s%d" % (name, i), shape, dt) for i in range(n)]
                self.i = 0

            def next(self):
                k = self.i % len(self.tiles)
                self.i += 1
                return self.tiles[k], "%s%d" % (self.name, k)

        def mm_acc(bank, lhs_fn, rhs_fn, KC, reads):
            n = rhs_fn(0).shape[-1]
            fns = [lambda e, kc=kc: e.matmul(out=pb[bank][:, 0:n], lhsT=lhs_fn(kc), rhs=rhs_fn(kc),
                                             start=(kc == 0), stop=(kc == KC - 1))
                   for kc in range(KC)]
            T.MM(reads, ["pb%d" % bank], fns)

        if stop is None or stop in ("C", "D", "E", "F", "G"):
            pcx = contextlib.ExitStack()
            h1 = sb(pcx, "h1", [128, 16, 2080], BF16)
            czv = S_all[:].rearrange("p a b c -> p (a b c)").rearrange("p (a c) -> p a c", a=8)
            with contextlib.ExitStack() as t0:
                xring = [sb(t0, "xringc%d" % i, [128, D], F32) for i in range(2)]
                xsb = [sb(t0, "xsbc%d" % i, [128, D], BF16) for i in range(2)]
                junk = sb(t0, "junkc", [128, D], BF16)
                for ti in range(-1, NT):
                    r = (ti + 1) % 2
                    src = xh[HIST - 128:HIST, :] if ti < 0 else xm[ti * 128:(ti + 1) * 128, :]
                    T.DMA("sp", "xr%d" % r, [], ["xr%d" % r], lambda e, r=r, src=src: e.dma_start(out=xring[r][:], in_=src))
                    rms_tile(xring[r][:], "xr%d" % r, xsb[r][:], "xs%d" % r, junk[:], "junk")
                    if ti < 0:
                        transpose_to_fm(xsb[r], "xs%d" % r, lambda half: h1[:, half * 8:(half + 1) * 8, 0:32], "h1",
                                        ncols=32, col0=96)
                    else:
                        transpose_to_fm(xsb[r], "xs%d" % r,
                                        lambda half, ti=ti: h1[:, half * 8:(half + 1) * 8, 32 + ti * 128:32 + (ti + 1) * 128], "h1")
                T.barrier()
            cblocks = [(0, 32)] + [(32 + b * 512, 512) for b in range(4)]
            with contextlib.ExitStack() as t0:
                wsr = Ring(t0, "wsc", 4, [128, 16, 128], BF16)
                sgb = sb(t0, "sgb", [128, 2080], BF16)
                zpr = Ring(t0, "zp", 2, [128, 2080], BF16)
                dgr = Ring(t0, "dg", 2, [128, 31, 128], BF16)
                for pc in range(8):
                    wa, wares = wsr.next()
                    wbt, wbres = wsr.next()
                    bg(3)
                    load_slab(wa[:], wares, wares, w_in[:, 1024 + pc * 128:1024 + (pc + 1) * 128], 16)
                    load_slab(wbt[:], wbres, wbres, w_in[:, 2048 + pc * 128:2048 + (pc + 1) * 128], 16)
                    zp, zres = zpr.next()
                    dg, dres = dgr.next()
                    T.I("dve", ["cst", "chp"], [dres], lambda e, dg=dg, pc=pc: e.tensor_tensor(
                        out=dg[:], in0=ident_f.unsqueeze(1).to_broadcast([128, 31, 128]),
                        in1=chp_t[:, pc, 4:35].unsqueeze(2).to_broadcast([128, 31, 128]), op=ALU.mult))
                    for ci, (c0, n) in enumerate(cblocks):
                        bk = (ci % 2) * 2
                        mm_acc(bk, lambda kc: wbt[:, kc, :], lambda kc: h1[:, kc, c0:c0 + n], 16, ["h1", wbres])
                        T.I("act", ["pb%d" % bk], ["sgb"], lambda e, bk=bk, c0=c0, n=n: e.activation(
                            out=sgb[:, c0:c0 + n], in_=pb[bk][:, 0:n], func=AF.Sigmoid))
                        mm_acc(bk + 1, lambda kc: wa[:, kc, :], lambda kc: h1[:, kc, c0:c0 + n], 16, ["h1", wares])
                        T.I("dve", ["pb%d" % (bk + 1), "sgb"], [zres], lambda e, bk=bk, c0=c0, n=n, zp=zp: e.tensor_tensor(
                            out=zp[:, c0:c0 + n], in0=pb[bk + 1][:, 0:n], in1=sgb[:, c0:c0 + n], op=ALU.mult))
                    for b in range(4):
                        bank = 4 + b % 2
                        T.MM([zres, dres], ["pb%d" % bank],
                             [lambda e, k=k, b=b, bank=bank, zp=zp, dg=dg: e.matmul(
                                 out=pb[bank][:, :], lhsT=dg[:, k, :], rhs=zp[:, 2 + k + b * 512:2 + k + (b + 1) * 512],
                                 start=(k == 0), stop=(k == 30)) for k in range(31)])
                        T.I("act", ["pb%d" % bank, "chp"], ["cz%d" % pc], lambda e, pc=pc, b=b, bank=bank: e.activation(
                            out=czv[:, pc, b * 512:(b + 1) * 512], in_=pb[bank][:, :], func=AF.Identity,
                            bias=chp_t[:, pc, 1:2]))
                T.barrier()
            with contextlib.ExitStack() as t0:
                sqr = Ring(t0, "sqt", 2, [128, 512], BF16)
                mean_t = sb(t0, "mean_t", [128, 512], F32)
                rstd_t = sb(t0, "rstd_t", [128, 512], F32)
                tmpf = Ring(t0, "tmpf", 2, [128, 512], F32)
                for b in range(4):
                    cs = slice(b * 512, (b + 1) * 512)
                    T.MM(["cz%d" % p for p in range(8)] + ["onesb"], ["pb0"],
                         [lambda e, p=p: e.matmul(out=pb[0][:, :], lhsT=onesb[:], rhs=czv[:, p, cs], start=(p == 0), stop=(p == 7))
                          for p in range(8)])
                    for p in range(8):
                        sq, sqres = sqr.next()
                        T.I("act", ["cz%d" % p], [sqres], lambda e, p=p, sq=sq: e.activation(out=sq[:], in_=czv[:, p, cs], func=AF.Square))
                        T.MM([sqres, "onesb"], ["pb1"], [lambda e, p=p, sq=sq: e.matmul(out=pb[1][:, :], lhsT=onesb[:], rhs=sq[:],
                                                                                   start=(p == 0), stop=(p == 7))])
                    T.I("dve", ["pb0"], ["mean_t"], lambda e: e.tensor_scalar(out=mean_t[:], in0=pb[0][:, :], scalar1=1.0 / 1024,
                                                                             scalar2=None, op0=ALU.mult))
                    T.I("dve", ["mean_t"], ["rstd_t"], lambda e: e.tensor_tensor(out=rstd_t[:], in0=mean_t[:], in1=mean_t[:], op=ALU.mult))
                    T.I("dve", ["pb1", "rstd_t"], ["rstd_t"], lambda e: e.scalar_tensor_tensor(
                        out=rstd_t[:], in0=pb[1][:, :], scalar=1.0 / 1024, in1=rstd_t[:], op0=ALU.mult, op1=ALU.subtract))
                    T.I("act", ["rstd_t"], ["rstd_t"], lambda e: e.activation(out=rstd_t[:], in_=rstd_t[:], func=AF.Sqrt,
                                                                              bias=epsc[:, 1:2]))
                    T.I("dve", ["rstd_t"], ["rstd_t"], lambda e: e.reciprocal(out=rstd_t[:], in_=rstd_t[:]))
                    for p in range(8):
                        tf, tfres = tmpf.next()
                        T.I("dve", ["cz%d" % p, "mean_t"], [tfres], lambda e, p=p, tf=tf: e.tensor_tensor(
                            out=tf[:], in0=czv[:, p, cs], in1=mean_t[:], op=ALU.subtract))
                        T.I("dve", [tfres, "rstd_t"], [tfres], lambda e, tf=tf: e.tensor_tensor(
                            out=tf[:], in0=tf[:], in1=rstd_t[:], op=ALU.mult))
                        T.I("act", [tfres, "chp"], ["cz%d" % p], lambda e, p=p, tf=tf: e.activation(
                            out=czv[:, p, cs], in_=tf[:], func=AF.Silu, scale=chp_t[:, p, 2:3], bias=chp_t[:, p, 3:4]))
                T.barrier()
            if stop == "C":
                dbg["zc2"] = (S_all, [128, 32, 2, NCH], BF16)

            if stop != "C":
                with contextlib.ExitStack() as t0:
                    w16 = Ring(t0, "wd16_", 4, [128, 16, 128], BF16)
                    w8 = Ring(t0, "wd8_", 6, [128, 8, 128], BF16)
                    sgr = Ring(t0, "sg", 6, [128, 512], BF16)
                    tr = Ring(t0, "tD", 4, [128, 512], BF16)
                    mrows = Ring(t0, "mrow", 2, [128, TC], BF16)
                    for j in range(16):
                        js = slice(j * 128, (j + 1) * 128)
                        wga, rga = w16.next()
                        wgb, rgb = w16.next()
                        wva, rva = w8.next()
                        wgt, rgt = w8.next()
                        wyb, ryb = w8.next()
                        load_slab(wga[:], rga, rga, w_in[:, 3072 + j * 128:3072 + (j + 1) * 128], 16)
                        load_slab(wgb[:], rgb, rgb, w_in[:, 5120 + j * 128:5120 + (j + 1) * 128], 16)
                        load_slab(wva[:], rva, rva, w_glu[:, js], 8)
                        load_slab(wgt[:], rgt, rgt, w_glu[:, 2048 + j * 128:2048 + (j + 1) * 128], 8)
                        load_slab(wyb[:], ryb, ryb, w_cwo[:, js], 8)
                        bg(1 + j % 2)
                        mrow, mres = mrows.next()
                        for b in range(4):
                            hs = slice(32 + b * 512, 32 + (b + 1) * 512)
                            cs = slice(b * 512, (b + 1) * 512)
                            mm_acc(0, lambda kc: wga[:, kc, :], lambda kc: h1[:, kc, hs], 16, ["h1", rga])
                            mm_acc(1, lambda kc: wgb[:, kc, :], lambda kc: h1[:, kc, hs], 16, ["h1", rgb])
                            zres = ["u%d_%d" % (p, b) for p in range(8)]
                            mm_acc(2, lambda kc: wva[:, kc, :], lambda kc: u_t[:, kc, cs], 8, zres + [rva])
                            mm_acc(3, lambda kc: wgt[:, kc, :], lambda kc: u_t[:, kc, cs], 8, zres + [rgt])
                            mm_acc(4, lambda kc: wyb[:, kc, :], lambda kc: czv[:, kc, cs], 8, ["cz%d" % p for p in range(8)] + [ryb])
                            s0, r0 = sgr.next()
                            s1, r1 = sgr.next()
                            s3, r3 = sgr.next()
                            T.I("act", ["pb0"], [r0], lambda e, s0=s0: e.activation(out=s0[:], in_=pb[0][:, :], func=AF.Sigmoid))
                            T.I("act", ["pb1"], [r1], lambda e, s1=s1: e.activation(out=s1[:], in_=pb[1][:, :], func=AF.Sigmoid))
                            T.I("act", ["pb3"], [r3], lambda e, s3=s3: e.activation(out=s3[:], in_=pb[3][:, :], func=AF.Sigmoid))
                            ta, rta = tr.next()
                            tb_, rtb = tr.next()
                            T.I("dve", ["pb2", r3], [rta], lambda e, ta=ta, s3=s3: e.tensor_tensor(out=ta[:], in0=pb[2][:, :], in1=s3[:], op=ALU.mult))
                            T.I("dve", [rta, r0], [rta], lambda e, ta=ta, s0=s0: e.tensor_tensor(out=ta[:], in0=ta[:], in1=s0[:], op=ALU.mult))
                            T.I("dve", ["pb4", r1], [rtb], lambda e, tb_=tb_, s1=s1: e.tensor_tensor(out=tb_[:], in0=pb[4][:, :], in1=s1[:], op=ALU.mult))
                            T.I("dve", [rta, rtb], [mres], lambda e, ta=ta, tb_=tb_, mrow=mrow, cs=cs: e.tensor_tensor(
                                out=mrow[:, cs], in0=ta[:], in1=tb_[:], op=ALU.add))
                        T.DMA("sp", mres, [mres], ["m_d"], lambda e, mrow=mrow, js=js: e.dma_start(out=m_d[js, :], in_=mrow[:]))
                    T.barrier()
            pcx.close()
        if stop is None or stop in ("E", "F", "G"):
            pu.close()


        def tm_residual(actT, ares, wsrc, xsrc_d, xdst_d, st, sfx):
            wsl = Ring(st, "wtm" + sfx, 2, [128, 16, 512], BF16)
            xts = Ring(st, "xtm" + sfx, 3, [128, 512], F32)
            for fb in range(4):
                fs = slice(fb * 512, (fb + 1) * 512)
                wt, wres = wsl.next()
                load_slab(wt[:], wres, wres, wsrc[:, fs], 16)
                bg(3 if sfx == "e" else 2)
                for ti in range(NT):
                    ts_ = slice(ti * 128, (ti + 1) * 128)
                    xt, xres = xts.next()
                    T.DMA("sp", xres, [], [xres], lambda e, xt=xt, ts_=ts_, fs=fs: e.dma_start(out=xt[:], in_=xsrc_d[ts_, fs]))
                    bank = ti % 4
                    mm_acc(bank, lambda kc: actT[:, kc, ts_], lambda kc: wt[:, kc, :], 16, [ares, wres])
                    T.I("dve", ["pb%d" % bank, xres], [xres], lambda e, xt=xt, bank=bank: e.tensor_tensor(
                        out=xt[:], in0=pb[bank][:, :], in1=xt[:], op=ALU.add))
                    T.DMA("sp", xres, [xres], [], lambda e, xt=xt, ts_=ts_, fs=fs: e.dma_start(out=xdst_d[ts_, fs], in_=xt[:]))
            T.barrier()

        if stop is None or stop in ("E", "F", "G"):
            pe_ = contextlib.ExitStack()
            bigA = sb(pe_, "bigA", [128, 16, TC], BF16)
            T.DMA("sp", "bigA", [], ["bigA"], lambda e: e.dma_start(out=bigA[:], in_=m_d.rearrange("(kc p) t -> p kc t", p=128)))
            with contextlib.ExitStack() as t0:
                tm_residual(bigA, "bigA", w_out, xm, x1_d, t0, "e")

            kT = sb(pe_, "kT", [128, 16, NMEM], BF16)
            v_t = sb(pe_, "v_t", [128, 2, D], BF16)
            with contextlib.ExitStack() as t0:
                memT = sb(t0, "memT", [128, 16, NMEM], BF16)
                xring = [sb(t0, "xringf%d" % i, [128, D], F32) for i in range(2)]
                xsb = [sb(t0, "xsbf%d" % i, [128, D], BF16) for i in range(2)]
                junk = sb(t0, "junkf", [128, D], BF16)
                wsl = Ring(t0, "wf0_", 2, [128, 16, 512], BF16)
                load_gain(2)
                for mt in range(2):
                    T.DMA("sp", "xr%d" % mt, [], ["xr%d" % mt], lambda e, mt=mt: e.dma_start(out=xring[mt][:], in_=memb[mt * 128:(mt + 1) * 128, :]))
                    rms_tile(xring[mt][:], "xr%d" % mt, xsb[mt][:], "xs%d" % mt, junk[:], "junk")
                    transpose_to_fm(xsb[mt], "xs%d" % mt, lambda half, mt=mt: memT[:, half * 8:(half + 1) * 8, mt * 128:(mt + 1) * 128], "memT")
                T.barrier()
                for s4 in range(4):
                    fs = slice(s4 * 512, (s4 + 1) * 512)
                    wt, wres = wsl.next()
                    load_slab(wt[:], wres, wres, w_k[:, fs], 16)
                    for fl in range(4):
                        bank = fl % 2
                        mm_acc(bank, lambda kc: wt[:, kc, fl * 128:(fl + 1) * 128], lambda kc: memT[:, kc, :], 16, ["memT", wres])
                        T.I("act", ["pb%d" % bank], ["kT"], lambda e, bank=bank, s4=s4, fl=fl: e.copy(
                            out=kT[:, s4 * 4 + fl, :], in_=pb[bank][:, 0:NMEM]))
                for s4 in range(4):
                    fs = slice(s4 * 512, (s4 + 1) * 512)
                    wt, wres = wsl.next()
                    load_slab(wt[:], wres, wres, w_v[:, fs], 16)
                    for mt in range(2):
                        bank = mt
                        mm_acc(bank, lambda kc: memT[:, kc, mt * 128:(mt + 1) * 128], lambda kc: wt[:, kc, :], 16, ["memT", wres])
                        T.I("act", ["pb%d" % bank], ["v_t"], lambda e, bank=bank, mt=mt, fs=fs: e.copy(out=v_t[:, mt, fs], in_=pb[bank][:, :]))
                T.barrier()
            bigB = sb(pe_, "bigB", [128, 16, TC], BF16)
            with contextlib.ExitStack() as t0:
                xring = [sb(t0, "xringg%d" % i, [128, D], F32) for i in range(2)]
                xsb = [sb(t0, "xsbg%d" % i, [128, D], BF16) for i in range(2)]
                junk = sb(t0, "junkg", [128, D], BF16)
                load_gain(1)
                for ti in range(NT):
                    r = ti % 2
                    T.DMA("sp", "xr%d" % r, [], ["xr%d" % r], lambda e, r=r, ti=ti: e.dma_start(out=xring[r][:], in_=x1_d[ti * 128:(ti + 1) * 128, :]))
                    rms_tile(xring[r][:], "xr%d" % r, xsb[r][:], "xs%d" % r, junk[:], "junk")
                    transpose_to_fm(xsb[r], "xs%d" % r, lambda half, ti=ti: bigA[:, half * 8:(half + 1) * 8, ti * 128:(ti + 1) * 128], "bigA")
                T.barrier()
            with contextlib.ExitStack() as t0:
                wsl = Ring(t0, "wf", 2, [128, 16, 512], BF16)
                qT = Ring(t0, "qT", 1, [128, 4, 512], BF16)
                Et = Ring(t0, "Et", 4, [128, 512], BF16)
                rinv = sb(t0, "rinv", [128, 512], F32)
                for hd in range(4):
                    wt, wres = wsl.next()
                    load_slab(wt[:], wres, wres, w_q[:, hd * 512:(hd + 1) * 512], 16)
                    bg(4)
                    for b in range(4):
                        cs = slice(b * 512, (b + 1) * 512)
                        qt, qres = qT.next()
                        for dl in range(4):
                            bank = dl % 2
                            mm_acc(bank, lambda kc: wt[:, kc, dl * 128:(dl + 1) * 128], lambda kc: bigA[:, kc, cs], 16, ["bigA", wres])
                            T.I("act", ["pb%d" % bank], [qres], lambda e, bank=bank, qt=qt, dl=dl: e.copy(out=qt[:, dl, :], in_=pb[bank][:, :]))
                        ets = []
                        for mc in range(2):
                            bank = 2 + mc
                            T.MM(["kT", qres], ["pb%d" % bank],
                                 [lambda e, dl=dl, mc=mc, bank=bank, qt=qt: e.matmul(
                                     out=pb[bank][:, :], lhsT=kT[:, hd * 4 + dl, mc * 128:(mc + 1) * 128], rhs=qt[:, dl, :],
                                     start=(dl == 0), stop=(dl == 3)) for dl in range(4)])
                            et, eres = Et.next()
                            T.I("act", ["pb%d" % bank], [eres], lambda e, bank=bank, et=et: e.activation(
                                out=et[:], in_=pb[bank][:, :], func=AF.Exp, scale=1.0 / math.sqrt(512.0)))
                            ets.append((et, eres))
                        T.MM([ets[0][1], ets[1][1], "onesb"], ["pb4"],
                             [lambda e, mc=mc: e.matmul(out=pb[4][:, :], lhsT=onesb[:], rhs=ets[mc][0][:], start=(mc == 0), stop=(mc == 1))
                              for mc in range(2)])
                        T.I("dve", ["pb4"], ["rinv"], lambda e: e.reciprocal(out=rinv[:], in_=pb[4][:, :]))
                        for dl in range(4):
                            bank = dl % 2
                            T.MM([ets[0][1], ets[1][1], "v_t"], ["pb%d" % bank],
                                 [lambda e, mc=mc, dl=dl, bank=bank: e.matmul(
                                     out=pb[bank][:, :], lhsT=v_t[:, mc, hd * 512 + dl * 128:hd * 512 + (dl + 1) * 128],
                                     rhs=ets[mc][0][:], start=(mc == 0), stop=(mc == 1)) for mc in range(2)])
                            T.I("dve", ["pb%d" % bank, "rinv"], ["bigB"], lambda e, bank=bank, dl=dl, cs=cs: e.tensor_tensor(
                                out=bigB[:, hd * 4 + dl, cs], in0=pb[bank][:, :], in1=rinv[:], op=ALU.mult))
                T.barrier()
            with contextlib.ExitStack() as t0:
                tm_residual(bigB, "bigB", w_o, x1_d, x2_d, t0, "g")
            pe_.close()


        if stop is None:
            phx = contextlib.ExitStack()
            idx_all = sb(phx, "idx_all", [128, NEXP], I32)
            w_all = sb(phx, "w_all", [128, NEXP], F32)
            accv = pb[5][:, 0:128].rearrange("p (x c) -> p x c", c=2)
            with contextlib.ExitStack() as t0:
                carry = sb(t0, "carry", [128, NEXP], F32)
                wr_sb = sb(t0, "wr_sb", [128, 16, 72], F32)
                brep_t = sb(t0, "brep_t", [128, 72], F32)
                xring = [sb(t0, "xringh%d" % i, [128, D], F32) for i in range(2)]
                h3b = [sb(t0, "h3b%d" % i, [128, D], BF16) for i in range(2)]
                junk = sb(t0, "junkh", [128, D], BF16)
                h3f = sb(t0, "h3f", [128, D], F32)
                h3Tf = sb(t0, "h3Tf", [128, 16, 128], F32)
                rt = sb(t0, "rt", [128, 400], F32)
                rt2 = sb(t0, "rt2", [128, 330], F32)
                Ab = sb(t0, "Ab", [128, NEXP], BF16)
                src4r = Ring(t0, "src4_", 4, [128, 2, 4], F32)
                ztb = sb(t0, "ztb", [128, 256], F32)
                T.I("dve", [], ["ztb"], lambda e: e.memset(ztb[:], 0.0))
                for _i in range(4):
                    _t, _r = src4r.next()
                    T.I("dve", [], [_r], lambda e, _t=_t: e.memset(_t[:], 0.0))
                T.DMA("sp", "tblz", ["ztb"], ["tblz"], lambda e: e.dma_start(
                    out=tbl_d.rearrange("(p a) c -> p (a c)", p=128), in_=ztb[:]))
                T.I("dve", [], ["carry"], lambda e: e.memset(carry[:], 0.0))
                T.DMA("sp", "c0", [], ["wr_sb"], lambda e: e.dma_start(out=wr_sb[:], in_=wr_d.rearrange("(kc p) n -> p kc n", p=128)))
                T.DMA("sp", "c0", [], ["brep"], lambda e: e.dma_start(out=brep_t[:], in_=brep[:, :]))
                load_gain(3)
                L = rt[:, 0:72]
                ohg = rt[:, 72:80]
                egt = rt[:, 80:88]
                tmp3 = rt[:, 88:152].rearrange("p (g e) -> p g e", e=8)
                fsel = rt[:, 152:160]
                oh1 = rt[:, 160:168]
                msk = rt[:, 168:176]
                oh2 = rt[:, 176:184]
                A1 = rt[:, 184:248]
                A2 = rt[:, 248:312]
                Aa = rt[:, 312:376]
                sc = lambda i: rt[:, 376 + i:377 + i]
                WA = rt2[:, 0:64]
                pos = rt2[:, 64:128]
                valid = rt2[:, 128:192]
                rowv = rt2[:, 192:256]
                tmpA = rt2[:, 256:320]

                def rd(fn, eng="dve"):
                    T.I(eng, ["rt", "cst", "brep", "carry"], ["rt"], fn)

                for ti in range(NT):
                    r = ti % 2
                    ts_ = slice(ti * 128, (ti + 1) * 128)
                    T.DMA("sp", "xr%d" % r, [], ["xr%d" % r], lambda e, r=r, ts_=ts_: e.dma_start(out=xring[r][:], in_=x2_d[ts_, :]))
                    rms_tile(xring[r][:], "xr%d" % r, h3f[:], "h3f", junk[:], "junk")
                    T.I("act", ["h3f"], ["h3b%d" % r], lambda e, r=r: e.copy(out=h3b[r][:], in_=h3f[:]))
                    T.DMA("sp", "h3b%d" % r, ["h3b%d" % r], ["h3_d"], lambda e, r=r, ts_=ts_: e.dma_start(out=h3_d[ts_, :], in_=h3b[r][:]))
                    bg(2)
                    for q4 in range(4):
                        bank = q4 % 2
                        T.MM(["h3f", "cst"], ["pb%d" % bank],
                             [lambda e, j=j, q4=q4, bank=bank: e.transpose(
                                 out=pb[bank][:, j * 128:(j + 1) * 128], in_=h3f[:, (q4 * 4 + j) * 128:(q4 * 4 + j + 1) * 128],
                                 identity=ident_f) for j in range(4)])
                        T.I("act" if bank == 0 else "dve", ["pb%d" % bank], ["h3Tf"],
                            (lambda e, q4=q4, bank=bank: e.copy(out=h3Tf[:, q4 * 4:(q4 + 1) * 4, :],
                                                                in_=pb[bank][:, :].rearrange("p (j c) -> p j c", c=128))) if bank == 0 else
                            (lambda e, q4=q4, bank=bank: e.tensor_copy(out=h3Tf[:, q4 * 4:(q4 + 1) * 4, :],
                                                                       in_=pb[bank][:, :].rearrange("p (j c) -> p j c", c=128))))
                    T.MM(["h3Tf", "wr_sb"], ["pb2"],
                         [lambda e, kc=kc: e.matmul(out=pb[2][:, 0:72], lhsT=h3Tf[:, kc, :], rhs=wr_sb[:, kc, :],
                                                    start=(kc == 0), stop=(kc == 15)) for kc in range(16)])
                    T.I("dve", ["pb2", "brep"], ["rt"], lambda e: e.tensor_tensor(out=L, in0=pb[2][:, 0:72], in1=brep_t[:], op=ALU.add))
                    rd(lambda e: e.reduce_max(out=sc(0), in_=rt[:, 0:8], axis=AX.X))
                    rd(lambda e: e.tensor_scalar(out=ohg, in0=rt[:, 0:8], scalar1=sc(0), scalar2=None, op0=ALU.is_equal))
                    rd(lambda e: e.tensor_scalar(out=sc(1), in0=sc(0), scalar1=-1.0, scalar2=None, op0=ALU.mult))
                    rd(lambda e: e.activation(out=egt, in_=rt[:, 0:8], func=AF.Exp, bias=sc(1), accum_out=sc(2)), "act")
                    rd(lambda e: e.reciprocal(out=sc(3), in_=sc(2)))
                    rd(lambda e: e.tensor_tensor(out=tmp3, in0=rt[:, 8:72].rearrange("p (g e) -> p g e", e=8),
                                                 in1=ohg.unsqueeze(2).to_broadcast([128, 8, 8]), op=ALU.mult))
                    rd(lambda e: e.tensor_reduce(out=fsel, in_=tmp3.rearrange("p g e -> p e g"), axis=AX.X, op=ALU.add))
                    rd(lambda e: e.reduce_max(out=sc(4), in_=fsel, axis=AX.X))
                    rd(lambda e: e.tensor_scalar(out=oh1, in0=fsel, scalar1=sc(4), scalar2=None, op0=ALU.is_equal))
                    rd(lambda e: e.scalar_tensor_tensor(out=msk, in0=oh1, scalar=-1e30, in1=fsel, op0=ALU.mult, op1=ALU.add))
                    rd(lambda e: e.reduce_max(out=sc(5), in_=msk, axis=AX.X))
                    rd(lambda e: e.tensor_scalar(out=oh2, in0=msk, scalar1=sc(5), scalar2=None, op0=ALU.is_equal))
                    rd(lambda e: e.tensor_tensor(out=sc(6), in0=sc(5), in1=sc(4), op=ALU.subtract))
                    rd(lambda e: e.activation(out=sc(7), in_=sc(6), func=AF.Exp), "act")
                    rd(lambda e: e.tensor_scalar(out=sc(8), in0=sc(7), scalar1=1.0, scalar2=None, op0=ALU.add))
                    rd(lambda e: e.reciprocal(out=sc(8), in_=sc(8)))
                    rd(lambda e: e.tensor_tensor(out=sc(9), in0=sc(3), in1=sc(8), op=ALU.mult))
                    rd(lambda e: e.tensor_tensor(out=sc(10), in0=sc(9), in1=sc(7), op=ALU.mult))
                    rd(lambda e: e.tensor_tensor(out=A1.rearrange("p (g e) -> p g e", e=8),
                                                 in0=ohg.unsqueeze(2).to_broadcast([128, 8, 8]),
                                                 in1=oh1.unsqueeze(1).to_broadcast([128, 8, 8]), op=ALU.mult))
                    rd(lambda e: e.tensor_tensor(out=A2.rearrange("p (g e) -> p g e", e=8),
                                                 in0=ohg.unsqueeze(2).to_broadcast([128, 8, 8]),
                                                 in1=oh2.unsqueeze(1).to_broadcast([128, 8, 8]), op=ALU.mult))
                    rd(lambda e: e.tensor_tensor(out=Aa, in0=A1, in1=A2, op=ALU.add))
                    T.I("dve", ["rt"], ["Ab"], lambda e: e.tensor_copy(out=Ab[:], in_=Aa))
                    T.I("dve", ["rt"], ["rt2"], lambda e: e.tensor_scalar(out=WA, in0=A1, scalar1=sc(9), scalar2=None, op0=ALU.mult))
                    T.I("dve", ["rt", "rt2"], ["rt2"], lambda e: e.scalar_tensor_tensor(out=WA, in0=A2, scalar=sc(10), in1=WA,
                                                                                        op0=ALU.mult, op1=ALU.add))
                    T.MM(["Ab", "trib"], ["pb3"], [lambda e: e.matmul(out=pb[3][:, 0:64], lhsT=trib[:], rhs=Ab[:], start=True, stop=True)])
                    T.MM(["Ab", "onesb"], ["pb4"], [lambda e: e.matmul(out=pb[4][:, 0:64], lhsT=onesb[:], rhs=Ab[:], start=True, stop=True)])
                    T.I("dve", ["pb3", "carry", "rt2"], ["rt2"], lambda e: e.tensor_tensor(out=pos, in0=pb[3][:, 0:64], in1=carry[:], op=ALU.add))
                    T.I("dve", ["pb4", "carry"], ["carry"], lambda e: e.tensor_tensor(out=carry[:], in0=pb[4][:, 0:64], in1=carry[:], op=ALU.add))

                    def r2(fn):
                        T.I("dve", ["rt", "rt2", "cst"], ["rt2", "rt"], fn)
                    r2(lambda e: e.tensor_scalar(out=valid, in0=pos, scalar1=128.0, scalar2=None, op0=ALU.is_lt))
                    r2(lambda e: e.tensor_tensor(out=rowv, in0=pos, in1=xcol, op=ALU.add))
                    r2(lambda e: e.scalar_tensor_tensor(out=rowv, in0=rowv, scalar=-8192.0, in1=valid, op0=ALU.add, op1=ALU.mult))
                    r2(lambda e: e.tensor_scalar(out=rowv, in0=rowv, scalar1=8192.0, scalar2=None, op0=ALU.add))
                    r2(lambda e: e.tensor_tensor(out=tmpA, in0=A1, in1=rowv, op=ALU.mult))
                    r2(lambda e: e.reduce_sum(out=sc(11), in_=tmpA, axis=AX.X))
                    r2(lambda e: e.tensor_tensor(out=tmpA, in0=A2, in1=rowv, op=ALU.mult))
                    r2(lambda e: e.reduce_sum(out=sc(12), in_=tmpA, axis=AX.X))
                    T.I("dve", ["rt"], ["slots"], lambda e, ti=ti: e.tensor_copy(out=slots_i[:, ti, :], in_=rt[:, 387:389]))
                    s4, s4res = src4r.next()
                    T.I("dve", ["cst"], [s4res], lambda e, ti=ti, s4=s4: e.tensor_copy(
                        out=s4[:, :, 0], in_=tokid[:, ti:ti + 1].to_broadcast([128, 2])))
                    T.I("dve", ["rt"], [s4res], lambda e, s4=s4: e.tensor_copy(out=s4[:, :, 1], in_=rt[:, 385:387]))
                    for k2 in range(2):
                        T.DMA("pool", "scat", [s4res, "slots", "tblz"], ["tbl_%d_%d" % (ti, k2)],
                              lambda e, s4=s4, ti=ti, k2=k2: e.indirect_dma_start(
                                  out=tbl_d[:, :], out_offset=bass.IndirectOffsetOnAxis(ap=slots_i[:, ti, k2:k2 + 1], axis=0),
                                  in_=s4[:, k2, :], in_offset=None, bounds_check=NEXP * 128 - 1, oob_is_err=False))
                tbl = sb(t0, "tbl", [128, NEXP, 4], F32)
                T.DMA("pool", "tbl", ["tbl_%d_%d" % (ti, k2) for ti in range(NT) for k2 in range(2)], ["tbl"],
                      lambda e: e.dma_start(out=tbl[:], in_=tbl_d.rearrange("(x s) c -> s x c", s=128)))
                T.I("dve", ["tbl"], ["idx_all"], lambda e: e.tensor_copy(out=idx_all[:], in_=tbl[:, :, 0]))
                T.I("dve", ["tbl"], ["w_all"], lambda e: e.tensor_copy(out=w_all[:], in_=tbl[:, :, 1]))
                T.barrier()

            bg(3 * NEXP)
            ds_bg = T.dsem["bg"]
            T.wr["ebf_all"] = ("d_bg", ds_bg[0], ds_bg[1])
            with contextlib.ExitStack() as t0:
                wgr = Ring(t0, "wg", 2, [128, 16, DE], BF16)
                wur = Ring(t0, "wu", 2, [128, 16, DE], BF16)
                wdr = Ring(t0, "wd", 2, [128, 4, D], BF16)
                hxr = Ring(t0, "hx", 2, [128, D], BF16)
                hxTr = Ring(t0, "hxT", 2, [128, 16, 128], BF16)
                sgr = Ring(t0, "sgx", 2, [128, DE], F32)
                ar = Ring(t0, "ax", 2, [128, DE], BF16)
                aTr = Ring(t0, "aTx", 2, [128, 4, 128], BF16)
                yr = Ring(t0, "yx", 2, [128, D], F32)
                zt = sb(t0, "zt", [128, D], F32)
                T.I("dve", [], ["zt"], lambda e: e.memset(zt[:], 0.0))
                T.DMA("sp", "zt", ["zt"], ["y_d"], lambda e: e.dma_start(out=y_d[NEXP * 128:NEXP * 128 + 128, :], in_=zt[:]))
                for x in range(NEXP):
                    wg, rg = wgr.next()
                    wu, ru = wur.next()
                    wd, rdn = wdr.next()
                    T.DMA("pool", rg, ["ebf_all"], [rg], lambda e, wg=wg, x=x: e.dma_start(
                        out=wg[:], in_=eg_bf[x, :, :].rearrange("p (kc n) -> p kc n", kc=16)))
                    T.DMA("pool", ru, ["ebf_all"], [ru], lambda e, wu=wu, x=x: e.dma_start(
                        out=wu[:], in_=eu_bf[x, :, :].rearrange("p (kc n) -> p kc n", kc=16)))
                    T.DMA("pool", rdn, ["ebf_all"], [rdn], lambda e, wd=wd, x=x: e.dma_start(
                        out=wd[:], in_=ed_bf[x, :, :].rearrange("p (kc n) -> p kc n", kc=4)))
                    hx, rhx = hxr.next()
                    T.DMA("pool", rhx, ["idx_all", "h3_d"], [rhx], lambda e, hx=hx, x=x: e.indirect_dma_start(
                        out=hx[:], out_offset=None, in_=h3_d[:, :],
                        in_offset=bass.IndirectOffsetOnAxis(ap=idx_all[:, x:x + 1], axis=0)))
                    hxT, rhxT = hxTr.next()
                    transpose_to_fm(hx, rhx, lambda half, hxT=hxT: hxT[:, half * 8:(half + 1) * 8, :], rhxT)
                    mm_acc(0, lambda kc: hxT[:, kc, :], lambda kc: wg[:, kc, :], 16, [rhxT, rg])
                    mm_acc(1, lambda kc: hxT[:, kc, :], lambda kc: wu[:, kc, :], 16, [rhxT, ru])
                    sg, rsg = sgr.next()
                    at, rat = ar.next()
                    T.I("act", ["pb0"], [rsg], lambda e, sg=sg: e.activation(out=sg[:], in_=pb[0][:, :], func=AF.Silu))
                    T.I("dve", ["pb1", rsg], [rat], lambda e, sg=sg, at=at: e.tensor_tensor(out=at[:], in0=pb[1][:, :], in1=sg[:], op=ALU.mult))
                    aT, raT = aTr.next()
                    T.MM([rat, "identb"], ["tp0"], [lambda e, j=j, at=at: e.transpose(
                        out=tp[0][:, j * 128:(j + 1) * 128], in_=at[:, j * 128:(j + 1) * 128], identity=identb[:]) for j in range(4)])
                    T.I("act", ["tp0"], [raT], lambda e, aT=aT: e.copy(out=aT[:], in_=tp[0][:, 0:512].rearrange("p (j c) -> p j c", c=128)))
                    yt, ry = yr.next()
                    for nb in range(4):
                        bank = 2 + nb % 3
                        mm_acc(bank, lambda kc: aT[:, kc, :], lambda kc: wd[:, kc, nb * 512:(nb + 1) * 512], 4, [raT, rdn])
                        T.I("dve" if nb % 2 == 0 else "act", ["pb%d" % bank, "w_all"], [ry],
                            (lambda e, yt=yt, nb=nb, bank=bank, x=x: e.tensor_scalar(
                                out=yt[:, nb * 512:(nb + 1) * 512], in0=pb[bank][:, :], scalar1=w_all[:, x:x + 1], scalar2=None,
                                op0=ALU.mult)) if nb % 2 == 0 else
                            (lambda e, yt=yt, nb=nb, bank=bank, x=x: e.activation(
                                out=yt[:, nb * 512:(nb + 1) * 512], in_=pb[bank][:, :], func=AF.Copy, scale=w_all[:, x:x + 1])))
                    T.DMA("sp", ry, [ry], ["y_d"], lambda e, yt=yt, x=x: e.dma_start(out=y_d[x * 128:(x + 1) * 128, :], in_=yt[:]))
                T.barrier()

            with contextlib.ExitStack() as t0:
                xr = Ring(t0, "xj", 2, [128, D], F32)
                g1r = Ring(t0, "g1j", 2, [128, D], F32)
                g2r = Ring(t0, "g2j", 2, [128, D], F32)
                orr = Ring(t0, "oj", 2, [128, D], F32)
                junk = sb(t0, "junkj", [128, D], BF16)
                load_gain(4)
                for ti in range(NT):
                    ts_ = slice(ti * 128, (ti + 1) * 128)
                    xt, rx = xr.next()
                    g1, r1 = g1r.next()
                    g2, r2_ = g2r.next()
                    ot, ro = orr.next()
                    T.DMA("sp", rx, [], [rx], lambda e, xt=xt, ts_=ts_: e.dma_start(out=xt[:], in_=x2_d[ts_, :]))
                    T.DMA("pool", r1, ["slots", "y_d"], [r1], lambda e, g1=g1, ti=ti: e.indirect_dma_start(
                        out=g1[:], out_offset=None, in_=y_d[:, :],
                        in_offset=bass.IndirectOffsetOnAxis(ap=slots_i[:, ti, 0:1], axis=0)))
                    T.DMA("pool", r2_, ["slots", "y_d"], [r2_], lambda e, g2=g2, ti=ti: e.indirect_dma_start(
                        out=g2[:], out_offset=None, in_=y_d[:, :],
                        in_offset=bass.IndirectOffsetOnAxis(ap=slots_i[:, ti, 1:2], axis=0)))
                    T.I("dve", [rx, r1], [rx], lambda e, xt=xt, g1=g1: e.tensor_tensor(out=xt[:], in0=xt[:], in1=g1[:], op=ALU.add))
                    T.I("dve", [rx, r2_], [rx], lambda e, xt=xt, g2=g2: e.tensor_tensor(out=xt[:], in0=xt[:], in1=g2[:], op=ALU.add))
                    rms_tile(xt[:], rx, ot[:], ro, junk[:], "junk")
                    T.DMA("sp", ro, [ro], ["out"], lambda e, ot=ot, ts_=ts_: e.dma_start(out=out_d[ts_, :], in_=ot[:]))
                T.barrier()
            phx.close()

        if stop is not None:
            if stop in ("E", "F", "G"):
                for name, src in (("x1", x1_d), ("x2", x2_d)):
                    dd = nc.dram_tensor("dbg_" + name, [TC, D], F32, kind="ExternalOutput").ap()
                    T.DMA("sp", "dbg", [], [], lambda e, dd=dd, src=src: e.dma_start(out=dd[:, :], in_=src[:, :]))
            for name, (tile, shape, dt) in dbg.items():
                dd = nc.dram_tensor("dbg_" + name, list(shape), dt, kind="ExternalOutput").ap()
                T.DMA("sp", "dbg", [name], [], lambda e, dd=dd, tile=tile: e.dma_start(out=dd, in_=tile[:]))
            T.barrier()
            pu.close()
            return nc

        T.barrier()
    return nc


def host_layout(inp):
    f = np.float32
    g = lambda k: np.asarray(inp[k])
    shared = {}
    shared["w_in"] = np.ascontiguousarray(g("w_in")[0])
    shared["w_glu"] = np.ascontiguousarray(g("s5_w_glu")[0])
    shared["w_cwo"] = np.ascontiguousarray(g("conv_w_out")[0])
    shared["w_out"] = np.ascontiguousarray(g("w_out")[0])
    shared["w_q"] = np.ascontiguousarray(g("xattn_wq")[0])
    shared["w_k"] = np.ascontiguousarray(g("xattn_wk")[0])
    shared["w_v"] = np.ascontiguousarray(g("xattn_wv")[0])
    shared["w_o"] = np.ascontiguousarray(g("xattn_wo")[0])
    gn = np.stack([g("norm_mix_g")[0], g("norm_xattn_g")[0], g("norm_mem_g")[0], g("norm_moe_g")[0],
                   g("norm_final_g")], 0)
    shared["gains"] = np.ascontiguousarray(np.broadcast_to(gn[:, None, :], (5, 128, D))).astype(f)
    def l128(a):
        return np.ascontiguousarray(a.reshape(32, 2, 64).transpose(1, 2, 0).reshape(128, 32))
    lre, lim = g("s5_lambda_re")[0], g("s5_lambda_im")[0]
    ldt = np.broadcast_to(g("s5_log_dt")[0][:, None], (64, 64))
    shared["lam3"] = np.ascontiguousarray(np.stack([l128(lre), l128(lim), l128(ldt)], 1)).astype(f)

    def expand(a):
        o = np.zeros((2, 64, 32, 2, 16), f)
        ar = a.reshape(32, 2, 64, 16)
        for par in range(2):
            o[par, :, :, par, :] = ar[:, par].transpose(1, 0, 2)
        return o.reshape(128, 32, 32)
    bre, bim = g("s5_b_re")[0], g("s5_b_im")[0]
    cre = g("s5_c_re")[0].transpose(0, 2, 1)
    cim = g("s5_c_im")[0].transpose(0, 2, 1)
    shared["bc4"] = np.ascontiguousarray(np.stack([expand(bre), expand(bim), expand(cre), expand(cim)], 1))
    chp = np.zeros((128, 8, 36), f)
    cm = lambda v: v.reshape(8, 128).T
    chp[:, :, 0] = cm(g("s5_d")[0])
    chp[:, :, 1] = cm(g("conv_dw_b")[0])
    chp[:, :, 2] = cm(g("conv_ln_g")[0])
    chp[:, :, 3] = cm(g("conv_ln_b")[0])
    chp[:, :, 4:35] = g("conv_dw_w")[0].T.reshape(8, 128, 31).transpose(1, 0, 2)
    shared["chp"] = chp
    wr = np.concatenate([g("router_w_group")[0], g("router_w_expert")[0].transpose(1, 0, 2).reshape(D, 64)], 1)
    shared["wr"] = np.ascontiguousarray(wr).astype(f)
    br = np.concatenate([g("router_b_group")[0], g("router_b_expert")[0].reshape(64)])
    shared["brep"] = np.ascontiguousarray(np.broadcast_to(br[None, :], (128, 72))).astype(f)
    shared["e_g"] = np.ascontiguousarray(g("exp_w_gate")[0])
    shared["e_u"] = np.ascontiguousarray(g("exp_w_up")[0])
    shared["e_d"] = np.ascontiguousarray(g("exp_w_down")[0])
    cst = np.zeros((128, 6, 128), f)
    cst[:, 0, :] = np.eye(128)
    cst[:, 1, :] = np.triu(np.ones((128, 128)), 1)
    cst[:, 2, :] = np.arange(128)[None, :]
    cst[:, 3, :16] = np.arange(16)[None, :] * 128 + np.arange(128)[:, None]
    cst[:, 4, :] = np.kron(np.eye(4), np.ones((32, 32)))
    cst[:, 5, :64] = np.arange(64)[None, :] * 128
    shared["cst"] = cst
    x, mem = g("x"), g("mem")
    per = []
    for c in range(NCORES):
        b, q = c // 4, c % 4
        xmc = np.ascontiguousarray(x[b, q * TC:(q + 1) * TC])
        xhc = np.zeros((HIST, D), f)
        if q > 0:
            xhc[HIST - q * TC:] = x[b, :q * TC]
        per.append({"xm": xmc, "xh": xhc, "memb": np.ascontiguousarray(mem[b])})
    return shared, per


def kernel(**inputs):
    shared, per = host_layout(inputs)
    nc = build_nc()
    in_maps = [dict(shared, **p) for p in per]
    res = run_bass_kernel_spmd(nc, in_maps, core_ids=list(range(NCORES)))
    out = np.zeros((2, 8192, D), np.float32)
    for c in range(NCORES):
        b, q = c // 4, c % 4
        out[b, q * TC:(q + 1) * TC] = res.results[c]["out"]
    return out
```

```python
import contextlib
import math
import numpy as np
import concourse.bass as bass
import concourse.mybir as mybir
from concourse.bass_utils import run_bass_kernel_spmd

F32 = mybir.dt.float32
BF16 = mybir.dt.bfloat16
I32 = mybir.dt.int32
AF = mybir.ActivationFunctionType
ALU = mybir.AluOpType
AX = mybir.AxisListType

NCORES = 8
D = 2048
TC = 2048
NT = TC // 128
HIST = 6144
NMEM = 256
NEXP = 64
DE = 512
QC = 8
NCH = TC // QC
RMS_EPS = 1e-6
LN_EPS = 1e-5
PI = math.pi


class Tracker:
    def __init__(self, nc, es):
        self.nc = nc
        self.es = es
        self.E = {}
        for name, obj in (("pe", nc.tensor), ("act", nc.scalar), ("dve", nc.vector),
                          ("pool", nc.gpsimd), ("sp", nc.sync)):
            sem = es.enter_context(nc.semaphore("sem_" + name))
            self.E[name] = dict(o=obj, sem=sem, n=0, seen={})
        self.wr = {}
        self.rd = {}
        self.dsem = {}

    def _wait(self, en, toks):
        e = self.E[en]
        for (k, sem, val) in toks:
            if e["seen"].get(k, 0) >= val:
                continue
            e["o"].wait_ge(sem, val)
            e["seen"][k] = val

    def _deps(self, reads, writes):
        toks = []
        for r in reads:
            if r in self.wr:
                toks.append(self.wr[r])
        for w in writes:
            if w in self.wr:
                toks.append(self.wr[w])
            toks.extend(self.rd.get(w, {}).values())
        return toks

    def _commit(self, tok, reads, writes):
        for r in reads:
            d = self.rd.setdefault(r, {})
            if tok[0] not in d or d[tok[0]][2] < tok[2]:
                d[tok[0]] = tok
        for w in writes:
            self.wr[w] = tok
            self.rd[w] = {}

    def I(self, en, reads, writes, fn):
        self._wait(en, self._deps(reads, writes))
        e = self.E[en]
        ins = fn(e["o"])
        e["n"] += 1
        ins.then_inc(e["sem"], 1)
        tok = (en, e["sem"], e["n"])
        self._commit(tok, reads, writes)
        return tok

    def MM(self, reads, writes, fns):
        self._wait("pe", self._deps(reads, writes))
        e = self.E["pe"]
        ins = None
        for f in fns:
            ins = f(e["o"])
        e["n"] += 1
        ins.then_inc(e["sem"], 1)
        tok = ("pe", e["sem"], e["n"])
        self._commit(tok, reads, writes)
        return tok

    def DMA(self, en, dname, reads, writes, fn):
        if dname not in self.dsem:
            self.dsem[dname] = [self.es.enter_context(self.nc.semaphore("d_" + dname)), 0]
        self._wait(en, self._deps(reads, writes))
        ds = self.dsem[dname]
        ins = fn(self.E[en]["o"])
        ds[1] += 16
        ins.then_inc(ds[0], 16)
        tok = ("d_" + dname, ds[0], ds[1])
        self._commit(tok, reads, writes)
        return tok

    def barrier(self):
        toks = [(n, e["sem"], e["n"]) for n, e in self.E.items() if e["n"] > 0]
        toks += [("d_" + k, v[0], v[1]) for k, v in self.dsem.items() if v[1] > 0]
        for en in self.E:
            self._wait(en, toks)
        self.wr = {}
        self.rd = {}


def build_nc(stop=None):
    nc = bass.Bass("TRN2", target_bir_lowering=False)

    def din(name, shape, dt=F32):
        return nc.dram_tensor(name, list(shape), dt, kind="ExternalInput").ap()

    xm = din("xm", [TC, D])
    xh = din("xh", [HIST, D])
    memb = din("memb", [NMEM, D])
    w_in = din("w_in", [D, 7168])
    w_glu = din("w_glu", [1024, 4096])
    w_cwo = din("w_cwo", [1024, D])
    w_out = din("w_out", [D, D])
    w_q = din("w_q", [D, D])
    w_k = din("w_k", [D, D])
    w_v = din("w_v", [D, D])
    w_o = din("w_o", [D, D])
    gains = din("gains", [5, 128, D])
    lam3 = din("lam3", [128, 3, 32])
    bc4 = din("bc4", [128, 4, 32, 32])
    chp = din("chp", [128, 8, 36])
    wr_d = din("wr", [D, 72])
    brep = din("brep", [128, 72])
    if stop is None:
        e_g = din("e_g", [NEXP, D, DE])
        e_u = din("e_u", [NEXP, D, DE])
        e_d = din("e_d", [NEXP, DE, D])
    cst = din("cst", [128, 6, 128])
    out_d = nc.dram_tensor("out", [TC, D], F32, kind="ExternalOutput").ap()

    def dscr(name, shape, dt):
        return nc.dram_tensor(name, list(shape), dt, kind="Internal").ap()

    m_d = dscr("m_d", [D, TC], BF16)
    x1_d = dscr("x1_d", [TC, D], F32)
    x2_d = dscr("x2_d", [TC, D], F32)
    h3_d = dscr("h3_d", [TC, D], BF16)
    y_d = dscr("y_d", [NEXP * 128 + 128, D], F32)
    if stop is None:
        eg_bf = dscr("eg_bf", [NEXP, 128, 16 * DE], BF16)
        tbl_d = dscr("tbl_d", [NEXP * 128, 4], F32)
        eu_bf = dscr("eu_bf", [NEXP, 128, 16 * DE], BF16)
        ed_bf = dscr("ed_bf", [NEXP, 128, 4 * D], BF16)

    dbg = {}

    with contextlib.ExitStack() as es:
        T = Tracker(nc, es)

        def sb(st, name, shape, dt):
            return st.enter_context(nc.sbuf_tensor(name, list(shape), dt))

        def pst(st, name, shape, dt):
            return st.enter_context(nc.psum_tensor(name, list(shape), dt))

        cst_t = sb(es, "cst_t", [128, 6, 128], F32)
        identb = sb(es, "identb", [128, 128], BF16)
        trib = sb(es, "trib", [128, 128], BF16)
        onesb = sb(es, "onesb", [128, 128], BF16)
        gb = sb(es, "gb", [128, D], F32)
        chp_t = sb(es, "chp_t", [128, 8, 36], F32)
        stat = sb(es, "stat", [128, 8], F32)
        epsc = sb(es, "epsc", [128, 2], F32)
        LamA = sb(es, "LamA", [128, 32, 2], F32)
        LamB = sb(es, "LamB", [128, 32, 2], F32)
        LamA64 = sb(es, "LamA64", [128, 32, 2], F32)
        LamB64 = sb(es, "LamB64", [128, 32, 2], F32)
        CRCI = sb(es, "CRCI", [128, 2, 32], F32)
        Xseg = sb(es, "Xseg", [128, 32, 2, 4], F32)
        sctS = sb(es, "sctS", [128, 2, 32, 2, 4], F32)
        PW = sb(es, "PW", [128, 9, 2, 32], F32)
        Xst = sb(es, "Xst", [128, 32, 2], F32)
        sct = sb(es, "sct", [128, 2, 32, 2], F32)
        slots_i = sb(es, "slots_i", [128, NT, 2], I32)
        pu = contextlib.ExitStack()
        u_t = sb(pu, "u_t", [128, 8, TC], BF16)
        S_all = sb(pu, "S_all", [128, 32, 2, NCH], BF16)

        pb = [pst(es, "pb%d" % i, [128, 512], F32) for i in range(6)]
        tp = [pst(es, "tp%d" % i, [128, 1024], BF16) for i in range(2)]

        ident_f = cst_t[:, 0, :]
        iota_row = cst_t[:, 2, :]
        tokid = cst_t[:, 3, :]
        bmask = cst_t[:, 4, :]
        xcol = cst_t[:, 5, 0:64]

        T.DMA("sp", "c0", [], ["cst"], lambda e: e.dma_start(out=cst_t[:], in_=cst[:, :, :]))
        T.DMA("sp", "c0", [], ["chp"], lambda e: e.dma_start(out=chp_t[:], in_=chp[:, :, :]))
        T.I("dve", ["cst"], ["identb"], lambda e: e.tensor_copy(out=identb[:], in_=ident_f))
        T.I("dve", ["cst"], ["trib"], lambda e: e.tensor_copy(out=trib[:], in_=cst_t[:, 1, :]))
        T.I("dve", [], ["onesb"], lambda e: e.memset(onesb[:], 1.0))
        T.I("dve", [], ["epsc"], lambda e: e.memset(epsc[:, 0:1], RMS_EPS))
        T.I("dve", [], ["epsc"], lambda e: e.memset(epsc[:, 1:2], LN_EPS))

        def load_gain(i):
            T.DMA("sp", "gain", [], ["gb"], lambda e: e.dma_start(out=gb[:], in_=gains[i, :, :]))

        def rms_tile(xt, xres, outt, ores, junk, jres, gres="gb"):
            T.I("act", [xres], [jres, "ssq"],
                lambda e: e.activation(out=junk, in_=xt, func=AF.Square, accum_out=stat[:, 0:1]))
            T.I("act", ["ssq"], ["rstd"],
                lambda e: e.activation(out=stat[:, 1:2], in_=stat[:, 0:1], func=AF.Sqrt, scale=1.0 / D,
                                       bias=epsc[:, 0:1]))
            T.I("dve", ["rstd"], ["rstd"], lambda e: e.reciprocal(out=stat[:, 1:2], in_=stat[:, 1:2]))
            T.I("dve", [xres, "rstd", gres], [ores],
                lambda e: e.scalar_tensor_tensor(out=outt, in0=xt, scalar=stat[:, 1:2], in1=gb[:],
                                                 op0=ALU.mult, op1=ALU.mult))

        def transpose_to_fm(src, sres, dst_fn, dres, ncols=128, col0=0):
            for half in range(2):
                fns = []
                for j in range(8):
                    kc = half * 8 + j
                    fns.append(lambda e, j=j, kc=kc: e.transpose(
                        out=tp[half][:, j * 128:(j + 1) * 128], in_=src[:, kc * 128:(kc + 1) * 128],
                        identity=identb[:]))
                T.MM([sres, "identb"], ["tp%d" % half], fns)
                view = tp[half][:].rearrange("p (j c) -> p j c", c=128)[:, :, col0:col0 + ncols]
                eng = "act" if half == 0 else "dve"
                if eng == "act":
                    T.I("act", ["tp%d" % half], [dres],
                        lambda e, view=view, half=half: e.copy(out=dst_fn(half), in_=view))
                else:
                    T.I("dve", ["tp%d" % half], [dres],
                        lambda e, view=view, half=half: e.tensor_copy(out=dst_fn(half), in_=view))

        bg_state = [0]

        def bg(n):
            if stop is not None:
                return
            for _ in range(n):
                i = bg_state[0]
                if i >= 3 * NEXP:
                    return
                bg_state[0] += 1
                x, which = i // 3, i % 3
                if which == 0:
                    src, dst, kcn = e_g[x, :, :], eg_bf[x, :, :], 16
                elif which == 1:
                    src, dst, kcn = e_u[x, :, :], eu_bf[x, :, :], 16
                else:
                    src, dst, kcn = e_d[x, :, :], ed_bf[x, :, :], 4
                T.DMA("pool", "bg", [], ["ebf%d" % i],
                      lambda e, src=src, dst=dst, kcn=kcn: e.dma_start(
                          out=dst.rearrange("p (kc n) -> p kc n", kc=kcn),
                          in_=src.rearrange("(kc p) n -> p kc n", p=128)))

        def load_slab(wbuf, wres, dname, src_ap, ncolsK):
            T.DMA("pool", dname, [], [wres],
                  lambda e: e.dma_start(out=wbuf, in_=src_ap.rearrange("(kc p) n -> p kc n", p=128)))

        ph = contextlib.ExitStack()
        sm = sb(ph, "sm", [128, 20, 32], F32)
        bc_t = sb(ph, "bc_t", [128, 4, 32, 32], F32)
        smi = sb(ph, "smi", [128, 32], I32)
        T.DMA("sp", "c0", [], ["sm"], lambda e: e.dma_start(out=sm[:, 0:3, :], in_=lam3[:, :, :]))
        T.DMA("sp", "c0", [], ["bc"], lambda e: e.dma_start(out=bc_t[:], in_=bc4[:, :, :, :]))
        S = lambda i: sm[:, i, :]
        LRE, LIM, LDT, DT, MAG, ANG, TMP, SN, CS, LBRE, LBIM, DEN, NR, CR, CI, T1, T2 = range(17)

        def dv(fn):
            T.I("dve", ["sm", "bc", "PW", "Bb"], ["sm"], fn)

        def ac(fn):
            T.I("act", ["sm"], ["sm"], fn)

        ac(lambda e: e.activation(out=S(DT), in_=S(LDT), func=AF.Exp))
        dv(lambda e: e.tensor_tensor(out=S(TMP), in0=S(LRE), in1=S(DT), op=ALU.mult))
        ac(lambda e: e.activation(out=S(MAG), in_=S(TMP), func=AF.Exp))
        dv(lambda e: e.tensor_tensor(out=S(ANG), in0=S(LIM), in1=S(DT), op=ALU.mult))
        def sin_of(dst, phase):
            dv(lambda e: e.tensor_scalar(out=S(T1), in0=S(ANG), scalar1=1.0 / (2.0 * PI), scalar2=8.5 + phase,
                                         op0=ALU.mult, op1=ALU.add))
            dv(lambda e: e.tensor_copy(out=smi[:], in_=S(T1)))
            dv(lambda e: e.tensor_copy(out=S(T2), in_=smi[:]))
            dv(lambda e: e.tensor_tensor(out=S(T1), in0=S(T1), in1=S(T2), op=ALU.subtract))
            dv(lambda e: e.scalar_tensor_tensor(out=S(T1), in0=S(T1), scalar=0.0, in1=S(T1),
                                                op0=ALU.is_lt, op1=ALU.add))
            dv(lambda e: e.tensor_scalar(out=S(T1), in0=S(T1), scalar1=2.0 * PI, scalar2=-PI,
                                         op0=ALU.mult, op1=ALU.add))
            ac(lambda e: e.activation(out=S(dst), in_=S(T1), func=AF.Sin))

        sin_of(SN, 0.0)
        sin_of(CS, 0.25)
        dv(lambda e: e.tensor_tensor(out=S(LBRE), in0=S(MAG), in1=S(CS), op=ALU.mult))
        dv(lambda e: e.tensor_tensor(out=S(LBIM), in0=S(MAG), in1=S(SN), op=ALU.mult))
        dv(lambda e: e.tensor_tensor(out=S(T1), in0=S(LRE), in1=S(LRE), op=ALU.mult))
        dv(lambda e: e.tensor_tensor(out=S(T2), in0=S(LIM), in1=S(LIM), op=ALU.mult))
        dv(lambda e: e.tensor_tensor(out=S(DEN), in0=S(T1), in1=S(T2), op=ALU.add))
        dv(lambda e: e.reciprocal(out=S(DEN), in_=S(DEN)))
        dv(lambda e: e.tensor_scalar(out=S(NR), in0=S(LBRE), scalar1=-1.0, scalar2=None, op0=ALU.add))
        dv(lambda e: e.tensor_tensor(out=S(T1), in0=S(NR), in1=S(LRE), op=ALU.mult))
        dv(lambda e: e.tensor_tensor(out=S(T2), in0=S(LBIM), in1=S(LIM), op=ALU.mult))
        dv(lambda e: e.tensor_tensor(out=S(T1), in0=S(T1), in1=S(T2), op=ALU.add))
        dv(lambda e: e.tensor_tensor(out=S(CR), in0=S(T1), in1=S(DEN), op=ALU.mult))
        dv(lambda e: e.tensor_tensor(out=S(T1), in0=S(LBIM), in1=S(LRE), op=ALU.mult))
        dv(lambda e: e.tensor_tensor(out=S(T2), in0=S(NR), in1=S(LIM), op=ALU.mult))
        dv(lambda e: e.tensor_tensor(out=S(T1), in0=S(T1), in1=S(T2), op=ALU.subtract))
        dv(lambda e: e.tensor_tensor(out=S(CI), in0=S(T1), in1=S(DEN), op=ALU.mult))

        def pwd(fn):
            T.I("dve", ["sm", "PW"], ["PW", "sm"], fn)

        pwd(lambda e: e.memset(PW[:, 0, 0, :], 1.0))
        pwd(lambda e: e.memset(PW[:, 0, 1, :], 0.0))
        for k in range(1, 9):
            pr_, pi_ = PW[:, k - 1, 0, :], PW[:, k - 1, 1, :]
            pwd(lambda e, pr_=pr_: e.tensor_tensor(out=S(T1), in0=pr_, in1=S(LBRE), op=ALU.mult))
            pwd(lambda e, pi_=pi_: e.tensor_tensor(out=S(T2), in0=pi_, in1=S(LBIM), op=ALU.mult))
            pwd(lambda e, k=k: e.tensor_tensor(out=PW[:, k, 0, :], in0=S(T1), in1=S(T2), op=ALU.subtract))
            pwd(lambda e, pr_=pr_: e.tensor_tensor(out=S(T1), in0=pr_, in1=S(LBIM), op=ALU.mult))
            pwd(lambda e, pi_=pi_: e.tensor_tensor(out=S(T2), in0=pi_, in1=S(LBRE), op=ALU.mult))
            pwd(lambda e, k=k: e.tensor_tensor(out=PW[:, k, 1, :], in0=S(T1), in1=S(T2), op=ALU.add))
        pwd(lambda e: e.tensor_copy(out=LamA[:, :, 0], in_=PW[:, 8, 0, :]))
        pwd(lambda e: e.tensor_copy(out=LamA[:, :, 1], in_=PW[:, 8, 0, :]))
        pwd(lambda e: e.tensor_scalar(out=LamB[:, :, 0], in0=PW[:, 8, 1, :], scalar1=-1.0, scalar2=None,
                                      op0=ALU.mult))
        pwd(lambda e: e.tensor_copy(out=LamB[:, :, 1], in_=PW[:, 8, 1, :]))
        pwd(lambda e: e.tensor_copy(out=S(17), in_=PW[:, 8, 0, :]))
        pwd(lambda e: e.tensor_copy(out=S(18), in_=PW[:, 8, 1, :]))
        for _sq in range(6):
            pwd(lambda e: e.tensor_tensor(out=S(T1), in0=S(17), in1=S(17), op=ALU.mult))
            pwd(lambda e: e.tensor_tensor(out=S(T2), in0=S(18), in1=S(18), op=ALU.mult))
            pwd(lambda e: e.tensor_tensor(out=S(T1), in0=S(T1), in1=S(T2), op=ALU.subtract))
            pwd(lambda e: e.tensor_tensor(out=S(T2), in0=S(17), in1=S(18), op=ALU.mult))
            pwd(lambda e: e.tensor_scalar(out=S(18), in0=S(T2), scalar1=2.0, scalar2=None, op0=ALU.mult))
            pwd(lambda e: e.tensor_copy(out=S(17), in_=S(T1)))
        pwd(lambda e: e.tensor_copy(out=LamA64[:, :, 0], in_=S(17)))
        pwd(lambda e: e.tensor_copy(out=LamA64[:, :, 1], in_=S(17)))
        pwd(lambda e: e.tensor_scalar(out=LamB64[:, :, 0], in0=S(18), scalar1=-1.0, scalar2=None, op0=ALU.mult))
        pwd(lambda e: e.tensor_copy(out=LamB64[:, :, 1], in_=S(18)))

        def bcast(ap32):
            return ap32.unsqueeze(2).to_broadcast([128, 32, 32])

        T.I("dve", ["sm"], ["CRCI"], lambda e: e.tensor_copy(out=CRCI[:, 0, :], in_=S(CR)))
        T.I("dve", ["sm"], ["CRCI"], lambda e: e.tensor_copy(out=CRCI[:, 1, :], in_=S(CI)))
        T.barrier()
        ph.close()

        def compute_Bb(st, sfx):
            Bb = sb(st, "Bb" + sfx, [128, 2, 32, 32], F32)
            with contextlib.ExitStack() as t1:
                bcB = sb(t1, "bcB" + sfx, [128, 2, 32, 32], F32)
                tA = sb(t1, "tAb" + sfx, [128, 32, 32], F32)
                tB = sb(t1, "tBb" + sfx, [128, 32, 32], F32)
                T.DMA("sp", "c0", [], ["bcB"], lambda e: e.dma_start(out=bcB[:], in_=bc4[:, 0:2, :, :]))
                cr_, ci_ = bcast(CRCI[:, 0, :]), bcast(CRCI[:, 1, :])

                def bd(fn):
                    T.I("dve", ["CRCI", "bcB", "tAb"], ["Bb", "tAb"], fn)
                bd(lambda e: e.tensor_tensor(out=tA[:], in0=bcB[:, 0], in1=cr_, op=ALU.mult))
                bd(lambda e: e.tensor_tensor(out=tB[:], in0=bcB[:, 1], in1=ci_, op=ALU.mult))
                bd(lambda e: e.tensor_tensor(out=Bb[:, 0], in0=tA[:], in1=tB[:], op=ALU.subtract))
                bd(lambda e: e.tensor_tensor(out=tA[:], in0=bcB[:, 1], in1=cr_, op=ALU.mult))
                bd(lambda e: e.tensor_tensor(out=tB[:], in0=bcB[:, 0], in1=ci_, op=ALU.mult))
                bd(lambda e: e.tensor_tensor(out=Bb[:, 1], in0=tA[:], in1=tB[:], op=ALU.add))
                T.barrier()
            return Bb

        def make_pk(k, Bb, Pb_, tA, tB):
            def f(fn):
                T.I("dve", ["Bb", "PW", "pk"], ["pk"], fn)
            pr_, pi_ = bcast(PW[:, k, 0, :]), bcast(PW[:, k, 1, :])
            f(lambda e: e.tensor_tensor(out=tA[:], in0=Bb[:, 0], in1=pr_, op=ALU.mult))
            f(lambda e: e.tensor_tensor(out=tB[:], in0=Bb[:, 1], in1=pi_, op=ALU.mult))
            f(lambda e: e.tensor_tensor(out=Pb_[:, 0], in0=tA[:], in1=tB[:], op=ALU.subtract))
            f(lambda e: e.tensor_tensor(out=tA[:], in0=Bb[:, 0], in1=pi_, op=ALU.mult))
            f(lambda e: e.tensor_tensor(out=tB[:], in0=Bb[:, 1], in1=pr_, op=ALU.mult))
            f(lambda e: e.tensor_tensor(out=Pb_[:, 1], in0=tA[:], in1=tB[:], op=ALU.add))

        pa = contextlib.ExitStack()
        Wu = sb(pa, "Wu", [128, 16, 1024], BF16)
        W2 = sb(pa, "W2", [128, 8, 8, 2, 128], BF16)

        load_slab(Wu[:, :, 0:512], "Wu", "wu0", w_in[:, 0:512], 16)
        load_slab(Wu[:, :, 512:1024], "Wu", "wu1", w_in[:, 512:1024], 16)
        load_gain(0)

        with contextlib.ExitStack() as t0:
            BbA = compute_Bb(t0, "A")
            Pb_ = sb(t0, "Pb_", [128, 2, 32, 32], BF16)
            tA = sb(t0, "tA2", [128, 32, 32], F32)
            tB = sb(t0, "tB2", [128, 32, 32], F32)
            for k in range(8):
                make_pk(k, BbA, Pb_, tA, tB)
                for ri in range(2):
                    fns = []
                    for pc in range(8):
                        fns.append(lambda e, pc=pc, ri=ri: e.transpose(
                            out=tp[ri][:, pc * 128:(pc + 1) * 128],
                            in_=Pb_[:, ri, pc * 4:(pc + 1) * 4, :].rearrange("p a b -> p (a b)"),
                            identity=identb[:]))
                    T.MM(["pk", "identb"], ["tp%d" % ri], fns)
                    T.I("act", ["tp%d" % ri], ["W2"],
                        lambda e, ri=ri, k=k: e.copy(out=W2[:, :, 7 - k, ri, :],
                                                     in_=tp[ri][:].rearrange("p (a b) -> p a b", b=128)))
            T.barrier()

        xring = [sb(pa, "xring%d" % i, [128, D], F32) for i in range(2)]
        xsb = [sb(pa, "xsb%d" % i, [128, D], BF16) for i in range(2)]
        h1blks = [sb(pa, "h1blk%d" % i, [128, 16, 512], BF16) for i in range(2)]
        T.I("dve", [], ["Xst"], lambda e: e.memset(Xst[:], 0.0))
        Xv = Xst[:]
        Xsw = bass.AP(Xv.tensor, Xv.offset + 1, [list(Xv.ap[0]), [2, 32], [-1, 2]])

        def scan_step(c, store=None, eng="dve"):
            Sc = S_all[:, :, :, c]
            T.I(eng, ["Xst"], ["sc0"], lambda e: e.tensor_tensor(out=sct[:, 0], in0=LamA[:], in1=Xst[:], op=ALU.mult))
            T.I(eng, ["Xst"], ["sc1"], lambda e: e.tensor_tensor(out=sct[:, 1], in0=LamB[:], in1=Xsw, op=ALU.mult))
            T.I(eng, ["sc0", "sc1"], ["sc0"], lambda e: e.tensor_tensor(out=sct[:, 0], in0=sct[:, 0], in1=sct[:, 1], op=ALU.add))
            T.I(eng, ["sc0", "S_all"], ["Xst"], lambda e: e.tensor_tensor(out=Xst[:], in0=sct[:, 0], in1=Sc, op=ALU.add))
            if store is not None:
                T.I("dve", ["Xst"], ["X_all"], lambda e: e.tensor_copy(out=store, in_=Xst[:]))

        Xg = Xseg[:]
        Xseg_sw = bass.AP(Xg.tensor, Xg.offset + 4, [list(Xg.ap[0]), [8, 32], [-4, 2], [1, 4]])
        LamA_b = LamA[:].unsqueeze(3).to_broadcast([128, 32, 2, 4])
        LamB_b = LamB[:].unsqueeze(3).to_broadcast([128, 32, 2, 4])

        def hist_scan():
            eng = "pool"
            T.I(eng, ["Xseg"], ["Xseg"], lambda e: e.memset(Xseg[:], 0.0))
            for c in range(64):
                Sv = S_all[:, :, :, c::64]
                T.I(eng, ["Xseg"], ["ss0"], lambda e: e.tensor_tensor(out=sctS[:, 0], in0=LamA_b, in1=Xseg[:], op=ALU.mult))
                T.I(eng, ["Xseg"], ["ss1"], lambda e: e.tensor_tensor(out=sctS[:, 1], in0=LamB_b, in1=Xseg_sw, op=ALU.mult))
                T.I(eng, ["ss0", "ss1"], ["ss0"], lambda e: e.tensor_tensor(out=sctS[:, 0], in0=sctS[:, 0], in1=sctS[:, 1], op=ALU.add))
                T.I(eng, ["ss0", "S_all"], ["Xseg"], lambda e, Sv=Sv: e.tensor_tensor(out=Xseg[:], in0=sctS[:, 0], in1=Sv, op=ALU.add))
            for sg_ in range(4):
                T.I(eng, ["Xst"], ["sc0"], lambda e: e.tensor_tensor(out=sct[:, 0], in0=LamA64[:], in1=Xst[:], op=ALU.mult))
                T.I(eng, ["Xst"], ["sc1"], lambda e: e.tensor_tensor(out=sct[:, 1], in0=LamB64[:], in1=Xsw, op=ALU.mult))
                T.I(eng, ["sc0", "sc1"], ["sc0"], lambda e: e.tensor_tensor(out=sct[:, 0], in0=sct[:, 0], in1=sct[:, 1], op=ALU.add))
                T.I(eng, ["sc0", "Xseg"], ["Xst"], lambda e, sg_=sg_: e.tensor_tensor(out=Xst[:], in0=sct[:, 0], in1=Xseg[:, :, :, sg_], op=ALU.add))

        tile_ctr = [0]
        blk_ctr = [0]
        for sbk in range(4):
            for blk in range(4):
                h1blk = h1blks[blk_ctr[0] % 2]
                h1res = "h1blk%d" % (blk_ctr[0] % 2)
                blk_ctr[0] += 1
                for t in range(4):
                    ti = blk * 4 + t
                    r = tile_ctr[0] % 2
                    tile_ctr[0] += 1
                    src = (xh[sbk * TC + ti * 128: sbk * TC + (ti + 1) * 128, :] if sbk < 3
                           else xm[ti * 128:(ti + 1) * 128, :])
                    T.DMA("sp", "xr%d" % r, [], ["xr%d" % r], lambda e, r=r, src=src: e.dma_start(out=xring[r][:], in_=src))
                    rms_tile(xring[r][:], "xr%d" % r, xsb[r][:], "xs%d" % r, xsb[r][:], "xs%d" % r)
                    transpose_to_fm(xsb[r], "xs%d" % r,
                                    lambda half, t=t, h1blk=h1blk: h1blk[:, half * 8:(half + 1) * 8, t * 128:(t + 1) * 128],
                                    h1res)
                bg(3)
                for pc in range(8):
                    bank = pc % 2
                    fns = [lambda e, kc=kc, pc=pc, bank=bank, h1blk=h1blk: e.matmul(
                        out=pb[bank][:, :], lhsT=Wu[:, kc, pc * 128:(pc + 1) * 128], rhs=h1blk[:, kc, :],
                        start=(kc == 0), stop=(kc == 15)) for kc in range(16)]
                    T.MM([h1res, "Wu"], ["pb%d" % bank], fns)
                    T.I("act", ["pb%d" % bank], ["u%d_%d" % (pc, blk)],
                        lambda e, pc=pc, blk=blk, bank=bank: e.copy(out=u_t[:, pc, blk * 512:(blk + 1) * 512], in_=pb[bank][:, :]))
            for prr in range(32):
                pc, a = prr // 4, prr % 4
                bank = 2 + prr % 2
                fns = []
                for ri in range(2):
                    for i in range(8):
                        fns.append(lambda e, ri=ri, i=i, pc=pc, a=a, bank=bank: e.matmul(
                            out=pb[bank][:, ri * NCH:(ri + 1) * NCH],
                            lhsT=W2[32 * a:32 * a + 32, pc, i, ri, :],
                            rhs=u_t[32 * a:32 * a + 32, pc, i::QC],
                            start=(i == 0), stop=(i == 7), tile_position=(32 * a, 0)))
                T.MM(["u%d_%d" % (pc, b) for b in range(4)] + ["W2"], ["pb%d" % bank], fns)
                T.I("act", ["pb%d" % bank], ["S_all"],
                    lambda e, prr=prr, bank=bank: e.copy(out=S_all[:, prr, :, :],
                                                         in_=pb[bank][:, :].rearrange("p (r c) -> p r c", r=2)))
            if sbk < 3:
                hist_scan()
        T.barrier()
        pa.close()

        if stop == "A":
            dbg["u"] = (u_t, [128, 8, TC], BF16)
            dbg["S_all"] = (S_all, [128, 32, 2, NCH], BF16)
            dbg["Xst"] = (Xst, [128, 32, 2], F32)

        if stop not in ("A",):
            pbx = contextlib.ExitStack()
            Kmat = sb(pbx, "Kmat", [128, 8, 8, 128], BF16)
            G = sb(pbx, "G", [128, 32, 8, 2, 32], BF16)
            X_all = sb(pbx, "X_all", [128, 32, 2, NCH], BF16)
            bc_t2 = sb(pbx, "bc_t2", [128, 2, 32, 32], F32)
            T.DMA("sp", "c0", [], ["bc2"], lambda e: e.dma_start(out=bc_t2[:], in_=bc4[:, 2:4, :, :]))
            with contextlib.ExitStack() as t0:
                BbB = compute_Bb(t0, "B")
                Pb_ = sb(t0, "Pb2_", [128, 2, 32, 32], BF16)
                Cb = sb(t0, "Cb", [128, 2, 32, 32], BF16)
                tA = sb(t0, "tA3", [128, 32, 32], F32)
                tB = sb(t0, "tB3", [128, 32, 32], F32)
                T.I("dve", ["bc2"], ["Cb"], lambda e: e.tensor_copy(out=Cb[:, 0], in_=bc_t2[:, 0]))
                T.I("dve", ["bc2"], ["Cb"], lambda e: e.tensor_scalar(out=Cb[:, 1], in0=bc_t2[:, 1], scalar1=-1.0,
                                                                      scalar2=None, op0=ALU.mult))
                for k in range(8):
                    make_pk(k, BbB, Pb_, tA, tB)
                    for pc in range(8):
                        bank = pc % 2
                        fns = [lambda e, ri=ri, pc=pc, bank=bank: e.matmul(
                            out=pb[bank][:, 0:128],
                            lhsT=Pb_[:, ri, pc * 4:(pc + 1) * 4, :].rearrange("p a b -> p (a b)"),
                            rhs=Cb[:, ri, pc * 4:(pc + 1) * 4, :].rearrange("p a b -> p (a b)"),
                            start=(ri == 0), stop=(ri == 1)) for ri in range(2)]
                        T.MM(["pk", "Cb"], ["pb%d" % bank], fns)
                        T.I("dve", ["pb%d" % bank, "cst"], ["Kmat"],
                            lambda e, pc=pc, k=k, bank=bank: e.tensor_tensor(
                                out=Kmat[:, pc, k, :], in0=pb[bank][:, 0:128], in1=bmask, op=ALU.mult))
                for pc in range(8):
                    T.I("dve", ["Kmat", "chp", "cst"], ["Kmat"],
                        lambda e, pc=pc: e.scalar_tensor_tensor(out=Kmat[:, pc, 0, :], in0=ident_f,
                                                                scalar=chp_t[:, pc, 0:1], in1=Kmat[:, pc, 0, :],
                                                                op0=ALU.mult, op1=ALU.add))
                for j in range(8):
                    pr_, pi_ = bcast(PW[:, j + 1, 0, :]), bcast(PW[:, j + 1, 1, :])

                    def g(fn):
                        T.I("dve", ["bc2", "PW", "gt"], ["G", "gt"], fn)
                    g(lambda e: e.tensor_tensor(out=tA[:], in0=bc_t2[:, 0], in1=pr_, op=ALU.mult))
                    g(lambda e: e.tensor_tensor(out=tB[:], in0=bc_t2[:, 1], in1=pi_, op=ALU.mult))
                    g(lambda e, j=j: e.tensor_tensor(out=G[:, :, j, 0, :], in0=tA[:], in1=tB[:], op=ALU.subtract))
                    g(lambda e: e.tensor_tensor(out=tA[:], in0=bc_t2[:, 0], in1=pi_, op=ALU.mult))
                    g(lambda e: e.tensor_tensor(out=tB[:], in0=bc_t2[:, 1], in1=pr_, op=ALU.mult))
                    g(lambda e: e.tensor_tensor(out=tA[:], in0=tA[:], in1=tB[:], op=ALU.add))
                    g(lambda e, j=j: e.tensor_scalar(out=G[:, :, j, 1, :], in0=tA[:], scalar1=-1.0, scalar2=None,
                                                     op0=ALU.mult))
                T.barrier()
            eng = "dve"
            Xs4 = sb(pbx, "Xs4", [128, 32, 2, 4], F32)
            X4 = Xs4[:]
            Xs4_sw = bass.AP(X4.tensor, X4.offset + 4, [list(X4.ap[0]), [8, 32], [-4, 2], [1, 4]])
            T.I(eng, ["Xseg"], ["Xseg"], lambda e: e.memset(Xseg[:], 0.0))
            for c in range(64):
                Sv = S_all[:, :, :, c::64]
                T.I(eng, ["Xseg"], ["ss0"], lambda e: e.tensor_tensor(out=sctS[:, 0], in0=LamA_b, in1=Xseg[:], op=ALU.mult))
                T.I(eng, ["Xseg"], ["ss1"], lambda e: e.tensor_tensor(out=sctS[:, 1], in0=LamB_b, in1=Xseg_sw, op=ALU.mult))
                T.I(eng, ["ss0", "ss1"], ["ss0"], lambda e: e.tensor_tensor(out=sctS[:, 0], in0=sctS[:, 0], in1=sctS[:, 1], op=ALU.add))
                T.I(eng, ["ss0", "S_all"], ["Xseg"], lambda e, Sv=Sv: e.tensor_tensor(out=Xseg[:], in0=sctS[:, 0], in1=Sv, op=ALU.add))
                if c + 1 < 64:
                    T.I(eng, ["Xseg"], ["X_all"], lambda e, c=c: e.tensor_copy(out=X_all[:, :, :, c + 1::64], in_=Xseg[:]))
            T.I(eng, ["Xst"], ["Xs4"], lambda e: e.tensor_copy(out=Xs4[:, :, :, 0], in_=Xst[:]))
            for sg_ in range(3):
                T.I(eng, ["Xst"], ["sc0"], lambda e: e.tensor_tensor(out=sct[:, 0], in0=LamA64[:], in1=Xst[:], op=ALU.mult))
                T.I(eng, ["Xst"], ["sc1"], lambda e: e.tensor_tensor(out=sct[:, 1], in0=LamB64[:], in1=Xsw, op=ALU.mult))
                T.I(eng, ["sc0", "sc1"], ["sc0"], lambda e: e.tensor_tensor(out=sct[:, 0], in0=sct[:, 0], in1=sct[:, 1], op=ALU.add))
                T.I(eng, ["sc0", "Xseg"], ["Xst"], lambda e, sg_=sg_: e.tensor_tensor(out=Xst[:], in0=sct[:, 0], in1=Xseg[:, :, :, sg_], op=ALU.add))
                T.I(eng, ["Xst"], ["Xs4"], lambda e, sg_=sg_: e.tensor_copy(out=Xs4[:, :, :, sg_ + 1], in_=Xst[:]))
            T.I(eng, ["Xs4"], ["X_all"], lambda e: e.tensor_copy(out=X_all[:, :, :, 0::64], in_=Xs4[:]))
            for k in range(1, 64):
                T.I(eng, ["Xs4"], ["ss0"], lambda e: e.tensor_tensor(out=sctS[:, 0], in0=LamA_b, in1=Xs4[:], op=ALU.mult))
                T.I(eng, ["Xs4"], ["ss1"], lambda e: e.tensor_tensor(out=sctS[:, 1], in0=LamB_b, in1=Xs4_sw, op=ALU.mult))
                T.I(eng, ["ss0", "ss1"], ["Xs4"], lambda e: e.tensor_tensor(out=Xs4[:], in0=sctS[:, 0], in1=sctS[:, 1], op=ALU.add))
                T.I(eng, ["Xs4", "X_all"], ["X_all"], lambda e, k=k: e.tensor_tensor(
                    out=X_all[:, :, :, k::64], in0=X_all[:, :, :, k::64], in1=Xs4[:], op=ALU.add))
            for pc in range(8):
                bg(3)
                for b in range(4):
                    bank = (pc * 4 + b) % 4
                    fns = []
                    for j in range(8):
                        for i in range(j + 1):
                            fns.append(lambda e, j=j, i=i, pc=pc, b=b, bank=bank: e.matmul(
                                out=pb[bank][:, j::QC], lhsT=Kmat[:, pc, j - i, :],
                                rhs=u_t[:, pc, b * 512 + i:(b + 1) * 512:QC],
                                start=(i == 0 and j == 0), stop=False))
                    for a in range(4):
                        prr = pc * 4 + a
                        for ri in range(2):
                            for j in range(8):
                                fns.append(lambda e, j=j, a=a, ri=ri, prr=prr, b=b, bank=bank: e.matmul(
                                    out=pb[bank][32 * a:32 * a + 32, j::QC], lhsT=G[:, prr, j, ri, :],
                                    rhs=X_all[:, prr, ri, b * 64:(b + 1) * 64],
                                    start=False, stop=(ri == 1 and j == 7), tile_position=(0, 32 * a)))
                    T.MM(["u%d_%d" % (pc, b), "Kmat", "G", "X_all"], ["pb%d" % bank], fns)
                    T.I("act", ["pb%d" % bank], ["u%d_%d" % (pc, b)],
                        lambda e, pc=pc, b=b, bank=bank: e.activation(
                            out=u_t[:, pc, b * 512:(b + 1) * 512], in_=pb[bank][:, :], func=AF.Gelu_apprx_tanh))
            T.barrier()
            if stop == "B":
                for name, tile, shape, dt in (("Kmat", Kmat, [128, 8, 8, 128], BF16), ("G", G, [128, 32, 8, 2, 32], BF16),
                                              ("X_all", X_all, [128, 32, 2, NCH], BF16)):
                    dd = nc.dram_tensor("dbg_" + name, list(shape), dt, kind="ExternalOutput").ap()
                    T.DMA("sp", "dbg", [], [], lambda e, dd=dd, tile=tile: e.dma_start(out=dd, in_=tile[:]))
                T.barrier()
            pbx.close()
            if stop == "B":
                dbg["zg"] = (u_t, [128, 8, TC], BF16)


        class Ring:
            def __init__(self, st, name, n, shape, dt):
                self.name = name
                self.tiles = [sb(st, "%s%d" % (name, i), shape, dt) for i in range(n)]
                self.i = 0

            def next(self):
                k = self.i % len(self.tiles)
                self.i += 1
                return self.tiles[k], "%s%d" % (self.name, k)

        def mm_acc(bank, lhs_fn, rhs_fn, KC, reads):
            n = rhs_fn(0).shape[-1]
            fns = [lambda e, kc=kc: e.matmul(out=pb[bank][:, 0:n], lhsT=lhs_fn(kc), rhs=rhs_fn(kc),
                                             start=(kc == 0), stop=(kc == KC - 1))
                   for kc in range(KC)]
            T.MM(reads, ["pb%d" % bank], fns)

        if stop is None or stop in ("C", "D", "E", "F", "G"):
            pcx = contextlib.ExitStack()
            h1 = sb(pcx, "h1", [128, 16, 2080], BF16)
            czv = S_all[:].rearrange("p a b c -> p (a b c)").rearrange("p (a c) -> p a c", a=8)
            with contextlib.ExitStack() as t0:
                xring = [sb(t0, "xringc%d" % i, [128, D], F32) for i in range(2)]
                xsb = [sb(t0, "xsbc%d" % i, [128, D], BF16) for i in range(2)]
                junk = sb(t0, "junkc", [128, D], BF16)
                for ti in range(-1, NT):
                    r = (ti + 1) % 2
                    src = xh[HIST - 128:HIST, :] if ti < 0 else xm[ti * 128:(ti + 1) * 128, :]
                    T.DMA("sp", "xr%d" % r, [], ["xr%d" % r], lambda e, r=r, src=src: e.dma_start(out=xring[r][:], in_=src))
                    rms_tile(xring[r][:], "xr%d" % r, xsb[r][:], "xs%d" % r, junk[:], "junk")
                    if ti < 0:
                        transpose_to_fm(xsb[r], "xs%d" % r, lambda half: h1[:, half * 8:(half + 1) * 8, 0:32], "h1",
                                        ncols=32, col0=96)
                    else:
                        transpose_to_fm(xsb[r], "xs%d" % r,
                                        lambda half, ti=ti: h1[:, half * 8:(half + 1) * 8, 32 + ti * 128:32 + (ti + 1) * 128], "h1")
                T.barrier()
            cblocks = [(0, 32)] + [(32 + b * 512, 512) for b in range(4)]
            with contextlib.ExitStack() as t0:
                wsr = Ring(t0, "wsc", 4, [128, 16, 128], BF16)
                sgb = sb(t0, "sgb", [128, 2080], BF16)
                zpr = Ring(t0, "zp", 2, [128, 2080], BF16)
                dgr = Ring(t0, "dg", 2, [128, 31, 128], BF16)
                for pc in range(8):
                    wa, wares = wsr.next()
                    wbt, wbres = wsr.next()
                    bg(3)
                    load_slab(wa[:], wares, wares, w_in[:, 1024 + pc * 128:1024 + (pc + 1) * 128], 16)
                    load_slab(wbt[:], wbres, wbres, w_in[:, 2048 + pc * 128:2048 + (pc + 1) * 128], 16)
                    zp, zres = zpr.next()
                    dg, dres = dgr.next()
                    T.I("dve", ["cst", "chp"], [dres], lambda e, dg=dg, pc=pc: e.tensor_tensor(
                        out=dg[:], in0=ident_f.unsqueeze(1).to_broadcast([128, 31, 128]),
                        in1=chp_t[:, pc, 4:35].unsqueeze(2).to_broadcast([128, 31, 128]), op=ALU.mult))
                    for ci, (c0, n) in enumerate(cblocks):
                        bk = (ci % 2) * 2
                        mm_acc(bk, lambda kc: wbt[:, kc, :], lambda kc: h1[:, kc, c0:c0 + n], 16, ["h1", wbres])
                        T.I("act", ["pb%d" % bk], ["sgb"], lambda e, bk=bk, c0=c0, n=n: e.activation(
                            out=sgb[:, c0:c0 + n], in_=pb[bk][:, 0:n], func=AF.Sigmoid))
                        mm_acc(bk + 1, lambda kc: wa[:, kc, :], lambda kc: h1[:, kc, c0:c0 + n], 16, ["h1", wares])
                        T.I("dve", ["pb%d" % (bk + 1), "sgb"], [zres], lambda e, bk=bk, c0=c0, n=n, zp=zp: e.tensor_tensor(
                            out=zp[:, c0:c0 + n], in0=pb[bk + 1][:, 0:n], in1=sgb[:, c0:c0 + n], op=ALU.mult))
                    for b in range(4):
                        bank = 4 + b % 2
                        T.MM([zres, dres], ["pb%d" % bank],
                             [lambda e, k=k, b=b, bank=bank, zp=zp, dg=dg: e.matmul(
                                 out=pb[bank][:, :], lhsT=dg[:, k, :], rhs=zp[:, 2 + k + b * 512:2 + k + (b + 1) * 512],
                                 start=(k == 0), stop=(k == 30)) for k in range(31)])
                        T.I("act", ["pb%d" % bank, "chp"], ["cz%d" % pc], lambda e, pc=pc, b=b, bank=bank: e.activation(
                            out=czv[:, pc, b * 512:(b + 1) * 512], in_=pb[bank][:, :], func=AF.Identity,
                            bias=chp_t[:, pc, 1:2]))
                T.barrier()
            with contextlib.ExitStack() as t0:
                sqr = Ring(t0, "sqt", 2, [128, 512], BF16)
                mean_t = sb(t0, "mean_t", [128, 512], F32)
                rstd_t = sb(t0, "rstd_t", [128, 512], F32)
                tmpf = Ring(t0, "tmpf", 2, [128, 512], F32)
                for b in range(4):
                    cs = slice(b * 512, (b + 1) * 512)
                    T.MM(["cz%d" % p for p in range(8)] + ["onesb"], ["pb0"],
                         [lambda e, p=p: e.matmul(out=pb[0][:, :], lhsT=onesb[:], rhs=czv[:, p, cs], start=(p == 0), stop=(p == 7))
                          for p in range(8)])
                    for p in range(8):
                        sq, sqres = sqr.next()
                        T.I("act", ["cz%d" % p], [sqres], lambda e, p=p, sq=sq: e.activation(out=sq[:], in_=czv[:, p, cs], func=AF.Square))
                        T.MM([sqres, "onesb"], ["pb1"], [lambda e, p=p, sq=sq: e.matmul(out=pb[1][:, :], lhsT=onesb[:], rhs=sq[:],
                                                                                   start=(p == 0), stop=(p == 7))])
                    T.I("dve", ["pb0"], ["mean_t"], lambda e: e.tensor_scalar(out=mean_t[:], in0=pb[0][:, :], scalar1=1.0 / 1024,
                                                                             scalar2=None, op0=ALU.mult))
                    T.I("dve", ["mean_t"], ["rstd_t"], lambda e: e.tensor_tensor(out=rstd_t[:], in0=mean_t[:], in1=mean_t[:], op=ALU.mult))
                    T.I("dve", ["pb1", "rstd_t"], ["rstd_t"], lambda e: e.scalar_tensor_tensor(
                        out=rstd_t[:], in0=pb[1][:, :], scalar=1.0 / 1024, in1=rstd_t[:], op0=ALU.mult, op1=ALU.subtract))
                    T.I("act", ["rstd_t"], ["rstd_t"], lambda e: e.activation(out=rstd_t[:], in_=rstd_t[:], func=AF.Sqrt,
                                                                              bias=epsc[:, 1:2]))
                    T.I("dve", ["rstd_t"], ["rstd_t"], lambda e: e.reciprocal(out=rstd_t[:], in_=rstd_t[:]))
                    for p in range(8):
                        tf, tfres = tmpf.next()
                        T.I("dve", ["cz%d" % p, "mean_t"], [tfres], lambda e, p=p, tf=tf: e.tensor_tensor(
                            out=tf[:], in0=czv[:, p, cs], in1=mean_t[:], op=ALU.subtract))
                        T.I("dve", [tfres, "rstd_t"], [tfres], lambda e, tf=tf: e.tensor_tensor(
                            out=tf[:], in0=tf[:], in1=rstd_t[:], op=ALU.mult))
                        T.I("act", [tfres, "chp"], ["cz%d" % p], lambda e, p=p, tf=tf: e.activation(
                            out=czv[:, p, cs], in_=tf[:], func=AF.Silu, scale=chp_t[:, p, 2:3], bias=chp_t[:, p, 3:4]))
                T.barrier()
            if stop == "C":
                dbg["zc2"] = (S_all, [128, 32, 2, NCH], BF16)

            if stop != "C":
                with contextlib.ExitStack() as t0:
                    w16 = Ring(t0, "wd16_", 4, [128, 16, 128], BF16)
                    w8 = Ring(t0, "wd8_", 6, [128, 8, 128], BF16)
                    sgr = Ring(t0, "sg", 6, [128, 512], BF16)
                    tr = Ring(t0, "tD", 4, [128, 512], BF16)
                    mrows = Ring(t0, "mrow", 2, [128, TC], BF16)
                    for j in range(16):
                        js = slice(j * 128, (j + 1) * 128)
                        wga, rga = w16.next()
                        wgb, rgb = w16.next()
                        wva, rva = w8.next()
                        wgt, rgt = w8.next()
                        wyb, ryb = w8.next()
                        load_slab(wga[:], rga, rga, w_in[:, 3072 + j * 128:3072 + (j + 1) * 128], 16)
                        load_slab(wgb[:], rgb, rgb, w_in[:, 5120 + j * 128:5120 + (j + 1) * 128], 16)
                        load_slab(wva[:], rva, rva, w_glu[:, js], 8)
                        load_slab(wgt[:], rgt, rgt, w_glu[:, 2048 + j * 128:2048 + (j + 1) * 128], 8)
                        load_slab(wyb[:], ryb, ryb, w_cwo[:, js], 8)
                        bg(1 + j % 2)
                        mrow, mres = mrows.next()
                        for b in range(4):
                            hs = slice(32 + b * 512, 32 + (b + 1) * 512)
                            cs = slice(b * 512, (b + 1) * 512)
                            mm_acc(0, lambda kc: wga[:, kc, :], lambda kc: h1[:, kc, hs], 16, ["h1", rga])
                            mm_acc(1, lambda kc: wgb[:, kc, :], lambda kc: h1[:, kc, hs], 16, ["h1", rgb])
                            zres = ["u%d_%d" % (p, b) for p in range(8)]
                            mm_acc(2, lambda kc: wva[:, kc, :], lambda kc: u_t[:, kc, cs], 8, zres + [rva])
                            mm_acc(3, lambda kc: wgt[:, kc, :], lambda kc: u_t[:, kc, cs], 8, zres + [rgt])
                            mm_acc(4, lambda kc: wyb[:, kc, :], lambda kc: czv[:, kc, cs], 8, ["cz%d" % p for p in range(8)] + [ryb])
                            s0, r0 = sgr.next()
                            s1, r1 = sgr.next()
                            s3, r3 = sgr.next()
                            T.I("act", ["pb0"], [r0], lambda e, s0=s0: e.activation(out=s0[:], in_=pb[0][:, :], func=AF.Sigmoid))
                            T.I("act", ["pb1"], [r1], lambda e, s1=s1: e.activation(out=s1[:], in_=pb[1][:, :], func=AF.Sigmoid))
                            T.I("act", ["pb3"], [r3], lambda e, s3=s3: e.activation(out=s3[:], in_=pb[3][:, :], func=AF.Sigmoid))
                            ta, rta = tr.next()
                            tb_, rtb = tr.next()
                            T.I("dve", ["pb2", r3], [rta], lambda e, ta=ta, s3=s3: e.tensor_tensor(out=ta[:], in0=pb[2][:, :], in1=s3[:], op=ALU.mult))
                            T.I("dve", [rta, r0], [rta], lambda e, ta=ta, s0=s0: e.tensor_tensor(out=ta[:], in0=ta[:], in1=s0[:], op=ALU.mult))
                            T.I("dve", ["pb4", r1], [rtb], lambda e, tb_=tb_, s1=s1: e.tensor_tensor(out=tb_[:], in0=pb[4][:, :], in1=s1[:], op=ALU.mult))
                            T.I("dve", [rta, rtb], [mres], lambda e, ta=ta, tb_=tb_, mrow=mrow, cs=cs: e.tensor_tensor(
                                out=mrow[:, cs], in0=ta[:], in1=tb_[:], op=ALU.add))
                        T.DMA("sp", mres, [mres], ["m_d"], lambda e, mrow=mrow, js=js: e.dma_start(out=m_d[js, :], in_=mrow[:]))
                    T.barrier()
            pcx.close()
        if stop is None or stop in ("E", "F", "G"):
            pu.close()


        def tm_residual(actT, ares, wsrc, xsrc_d, xdst_d, st, sfx):
            wsl = Ring(st, "wtm" + sfx, 2, [128, 16, 512], BF16)
            xts = Ring(st, "xtm" + sfx, 3, [128, 512], F32)
            for fb in range(4):
                fs = slice(fb * 512, (fb + 1) * 512)
                wt, wres = wsl.next()
                load_slab(wt[:], wres, wres, wsrc[:, fs], 16)
                bg(3 if sfx == "e" else 2)
                for ti in range(NT):
                    ts_ = slice(ti * 128, (ti + 1) * 128)
                    xt, xres = xts.next()
                    T.DMA("sp", xres, [], [xres], lambda e, xt=xt, ts_=ts_, fs=fs: e.dma_start(out=xt[:], in_=xsrc_d[ts_, fs]))
                    bank = ti % 4
                    mm_acc(bank, lambda kc: actT[:, kc, ts_], lambda kc: wt[:, kc, :], 16, [ares, wres])
                    T.I("dve", ["pb%d" % bank, xres], [xres], lambda e, xt=xt, bank=bank: e.tensor_tensor(
                        out=xt[:], in0=pb[bank][:, :], in1=xt[:], op=ALU.add))
                    T.DMA("sp", xres, [xres], [], lambda e, xt=xt, ts_=ts_, fs=fs: e.dma_start(out=xdst_d[ts_, fs], in_=xt[:]))
            T.barrier()

        if stop is None or stop in ("E", "F", "G"):
            pe_ = contextlib.ExitStack()
            bigA = sb(pe_, "bigA", [128, 16, TC], BF16)
            T.DMA("sp", "bigA", [], ["bigA"], lambda e: e.dma_start(out=bigA[:], in_=m_d.rearrange("(kc p) t -> p kc t", p=128)))
            with contextlib.ExitStack() as t0:
                tm_residual(bigA, "bigA", w_out, xm, x1_d, t0, "e")

            kT = sb(pe_, "kT", [128, 16, NMEM], BF16)
            v_t = sb(pe_, "v_t", [128, 2, D], BF16)
            with contextlib.ExitStack() as t0:
                memT = sb(t0, "memT", [128, 16, NMEM], BF16)
                xring = [sb(t0, "xringf%d" % i, [128, D], F32) for i in range(2)]
                xsb = [sb(t0, "xsbf%d" % i, [128, D], BF16) for i in range(2)]
                junk = sb(t0, "junkf", [128, D], BF16)
                wsl = Ring(t0, "wf0_", 2, [128, 16, 512], BF16)
                load_gain(2)
                for mt in range(2):
                    T.DMA("sp", "xr%d" % mt, [], ["xr%d" % mt], lambda e, mt=mt: e.dma_start(out=xring[mt][:], in_=memb[mt * 128:(mt + 1) * 128, :]))
                    rms_tile(xring[mt][:], "xr%d" % mt, xsb[mt][:], "xs%d" % mt, junk[:], "junk")
                    transpose_to_fm(xsb[mt], "xs%d" % mt, lambda half, mt=mt: memT[:, half * 8:(half + 1) * 8, mt * 128:(mt + 1) * 128], "memT")
                T.barrier()
                for s4 in range(4):
                    fs = slice(s4 * 512, (s4 + 1) * 512)
                    wt, wres = wsl.next()
                    load_slab(wt[:], wres, wres, w_k[:, fs], 16)
                    for fl in range(4):
                        bank = fl % 2
                        mm_acc(bank, lambda kc: wt[:, kc, fl * 128:(fl + 1) * 128], lambda kc: memT[:, kc, :], 16, ["memT", wres])
                        T.I("act", ["pb%d" % bank], ["kT"], lambda e, bank=bank, s4=s4, fl=fl: e.copy(
                            out=kT[:, s4 * 4 + fl, :], in_=pb[bank][:, 0:NMEM]))
                for s4 in range(4):
                    fs = slice(s4 * 512, (s4 + 1) * 512)
                    wt, wres = wsl.next()
                    load_slab(wt[:], wres, wres, w_v[:, fs], 16)
                    for mt in range(2):
                        bank = mt
                        mm_acc(bank, lambda kc: memT[:, kc, mt * 128:(mt + 1) * 128], lambda kc: wt[:, kc, :], 16, ["memT", wres])
                        T.I("act", ["pb%d" % bank], ["v_t"], lambda e, bank=bank, mt=mt, fs=fs: e.copy(out=v_t[:, mt, fs], in_=pb[bank][:, :]))
                T.barrier()
            bigB = sb(pe_, "bigB", [128, 16, TC], BF16)
            with contextlib.ExitStack() as t0:
                xring = [sb(t0, "xringg%d" % i, [128, D], F32) for i in range(2)]
                xsb = [sb(t0, "xsbg%d" % i, [128, D], BF16) for i in range(2)]
                junk = sb(t0, "junkg", [128, D], BF16)
                load_gain(1)
                for ti in range(NT):
                    r = ti % 2
                    T.DMA("sp", "xr%d" % r, [], ["xr%d" % r], lambda e, r=r, ti=ti: e.dma_start(out=xring[r][:], in_=x1_d[ti * 128:(ti + 1) * 128, :]))
                    rms_tile(xring[r][:], "xr%d" % r, xsb[r][:], "xs%d" % r, junk[:], "junk")
                    transpose_to_fm(xsb[r], "xs%d" % r, lambda half, ti=ti: bigA[:, half * 8:(half + 1) * 8, ti * 128:(ti + 1) * 128], "bigA")
                T.barrier()
            with contextlib.ExitStack() as t0:
                wsl = Ring(t0, "wf", 2, [128, 16, 512], BF16)
                qT = Ring(t0, "qT", 1, [128, 4, 512], BF16)
                Et = Ring(t0, "Et", 4, [128, 512], BF16)
                rinv = sb(t0, "rinv", [128, 512], F32)
                for hd in range(4):
                    wt, wres = wsl.next()
                    load_slab(wt[:], wres, wres, w_q[:, hd * 512:(hd + 1) * 512], 16)
                    bg(4)
                    for b in range(4):
                        cs = slice(b * 512, (b + 1) * 512)
                        qt, qres = qT.next()
                        for dl in range(4):
                            bank = dl % 2
                            mm_acc(bank, lambda kc: wt[:, kc, dl * 128:(dl + 1) * 128], lambda kc: bigA[:, kc, cs], 16, ["bigA", wres])
                            T.I("act", ["pb%d" % bank], [qres], lambda e, bank=bank, qt=qt, dl=dl: e.copy(out=qt[:, dl, :], in_=pb[bank][:, :]))
                        ets = []
                        for mc in range(2):
                            bank = 2 + mc
                            T.MM(["kT", qres], ["pb%d" % bank],
                                 [lambda e, dl=dl, mc=mc, bank=bank, qt=qt: e.matmul(
                                     out=pb[bank][:, :], lhsT=kT[:, hd * 4 + dl, mc * 128:(mc + 1) * 128], rhs=qt[:, dl, :],
                                     start=(dl == 0), stop=(dl == 3)) for dl in range(4)])
                            et, eres = Et.next()
                            T.I("act", ["pb%d" % bank], [eres], lambda e, bank=bank, et=et: e.activation(
                                out=et[:], in_=pb[bank][:, :], func=AF.Exp, scale=1.0 / math.sqrt(512.0)))
                            ets.append((et, eres))
                        T.MM([ets[0][1], ets[1][1], "onesb"], ["pb4"],
                             [lambda e, mc=mc: e.matmul(out=pb[4][:, :], lhsT=onesb[:], rhs=ets[mc][0][:], start=(mc == 0), stop=(mc == 1))
                              for mc in range(2)])
                        T.I("dve", ["pb4"], ["rinv"], lambda e: e.reciprocal(out=rinv[:], in_=pb[4][:, :]))
                        for dl in range(4):
                            bank = dl % 2
                            T.MM([ets[0][1], ets[1][1], "v_t"], ["pb%d" % bank],
                                 [lambda e, mc=mc, dl=dl, bank=bank: e.matmul(
                                     out=pb[bank][:, :], lhsT=v_t[:, mc, hd * 512 + dl * 128:hd * 512 + (dl + 1) * 128],
                                     rhs=ets[mc][0][:], start=(mc == 0), stop=(mc == 1)) for mc in range(2)])
                            T.I("dve", ["pb%d" % bank, "rinv"], ["bigB"], lambda e, bank=bank, dl=dl, cs=cs: e.tensor_tensor(
                                out=bigB[:, hd * 4 + dl, cs], in0=pb[bank][:, :], in1=rinv[:], op=ALU.mult))
                T.barrier()
            with contextlib.ExitStack() as t0:
                tm_residual(bigB, "bigB", w_o, x1_d, x2_d, t0, "g")
            pe_.close()


        if stop is None:
            phx = contextlib.ExitStack()
            idx_all = sb(phx, "idx_all", [128, NEXP], I32)
            w_all = sb(phx, "w_all", [128, NEXP], F32)
            accv = pb[5][:, 0:128].rearrange("p (x c) -> p x c", c=2)
            with contextlib.ExitStack() as t0:
                carry = sb(t0, "carry", [128, NEXP], F32)
                wr_sb = sb(t0, "wr_sb", [128, 16, 72], F32)
                brep_t = sb(t0, "brep_t", [128, 72], F32)
                xring = [sb(t0, "xringh%d" % i, [128, D], F32) for i in range(2)]
                h3b = [sb(t0, "h3b%d" % i, [128, D], BF16) for i in range(2)]
                junk = sb(t0, "junkh", [128, D], BF16)
                h3f = sb(t0, "h3f", [128, D], F32)
                h3Tf = sb(t0, "h3Tf", [128, 16, 128], F32)
                rt = sb(t0, "rt", [128, 400], F32)
                rt2 = sb(t0, "rt2", [128, 330], F32)
                Ab = sb(t0, "Ab", [128, NEXP], BF16)
                src4r = Ring(t0, "src4_", 4, [128, 2, 4], F32)
                ztb = sb(t0, "ztb", [128, 256], F32)
                T.I("dve", [], ["ztb"], lambda e: e.memset(ztb[:], 0.0))
                for _i in range(4):
                    _t, _r = src4r.next()
                    T.I("dve", [], [_r], lambda e, _t=_t: e.memset(_t[:], 0.0))
                T.DMA("sp", "tblz", ["ztb"], ["tblz"], lambda e: e.dma_start(
                    out=tbl_d.rearrange("(p a) c -> p (a c)", p=128), in_=ztb[:]))
                T.I("dve", [], ["carry"], lambda e: e.memset(carry[:], 0.0))
                T.DMA("sp", "c0", [], ["wr_sb"], lambda e: e.dma_start(out=wr_sb[:], in_=wr_d.rearrange("(kc p) n -> p kc n", p=128)))
                T.DMA("sp", "c0", [], ["brep"], lambda e: e.dma_start(out=brep_t[:], in_=brep[:, :]))
                load_gain(3)
                L = rt[:, 0:72]
                ohg = rt[:, 72:80]
                egt = rt[:, 80:88]
                tmp3 = rt[:, 88:152].rearrange("p (g e) -> p g e", e=8)
                fsel = rt[:, 152:160]
                oh1 = rt[:, 160:168]
                msk = rt[:, 168:176]
                oh2 = rt[:, 176:184]
                A1 = rt[:, 184:248]
                A2 = rt[:, 248:312]
                Aa = rt[:, 312:376]
                sc = lambda i: rt[:, 376 + i:377 + i]
                WA = rt2[:, 0:64]
                pos = rt2[:, 64:128]
                valid = rt2[:, 128:192]
                rowv = rt2[:, 192:256]
                tmpA = rt2[:, 256:320]

                def rd(fn, eng="dve"):
                    T.I(eng, ["rt", "cst", "brep", "carry"], ["rt"], fn)

                for ti in range(NT):
                    r = ti % 2
                    ts_ = slice(ti * 128, (ti + 1) * 128)
                    T.DMA("sp", "xr%d" % r, [], ["xr%d" % r], lambda e, r=r, ts_=ts_: e.dma_start(out=xring[r][:], in_=x2_d[ts_, :]))
                    rms_tile(xring[r][:], "xr%d" % r, h3f[:], "h3f", junk[:], "junk")
                    T.I("act", ["h3f"], ["h3b%d" % r], lambda e, r=r: e.copy(out=h3b[r][:], in_=h3f[:]))
                    T.DMA("sp", "h3b%d" % r, ["h3b%d" % r], ["h3_d"], lambda e, r=r, ts_=ts_: e.dma_start(out=h3_d[ts_, :], in_=h3b[r][:]))
                    bg(2)
                    for q4 in range(4):
                        bank = q4 % 2
                        T.MM(["h3f", "cst"], ["pb%d" % bank],
                             [lambda e, j=j, q4=q4, bank=bank: e.transpose(
                                 out=pb[bank][:, j * 128:(j + 1) * 128], in_=h3f[:, (q4 * 4 + j) * 128:(q4 * 4 + j + 1) * 128],
                                 identity=ident_f) for j in range(4)])
                        T.I("act" if bank == 0 else "dve", ["pb%d" % bank], ["h3Tf"],
                            (lambda e, q4=q4, bank=bank: e.copy(out=h3Tf[:, q4 * 4:(q4 + 1) * 4, :],
                                                                in_=pb[bank][:, :].rearrange("p (j c) -> p j c", c=128))) if bank == 0 else
                            (lambda e, q4=q4, bank=bank: e.tensor_copy(out=h3Tf[:, q4 * 4:(q4 + 1) * 4, :],
                                                                       in_=pb[bank][:, :].rearrange("p (j c) -> p j c", c=128))))
                    T.MM(["h3Tf", "wr_sb"], ["pb2"],
                         [lambda e, kc=kc: e.matmul(out=pb[2][:, 0:72], lhsT=h3Tf[:, kc, :], rhs=wr_sb[:, kc, :],
                                                    start=(kc == 0), stop=(kc == 15)) for kc in range(16)])
                    T.I("dve", ["pb2", "brep"], ["rt"], lambda e: e.tensor_tensor(out=L, in0=pb[2][:, 0:72], in1=brep_t[:], op=ALU.add))
                    rd(lambda e: e.reduce_max(out=sc(0), in_=rt[:, 0:8], axis=AX.X))
                    rd(lambda e: e.tensor_scalar(out=ohg, in0=rt[:, 0:8], scalar1=sc(0), scalar2=None, op0=ALU.is_equal))
                    rd(lambda e: e.tensor_scalar(out=sc(1), in0=sc(0), scalar1=-1.0, scalar2=None, op0=ALU.mult))
                    rd(lambda e: e.activation(out=egt, in_=rt[:, 0:8], func=AF.Exp, bias=sc(1), accum_out=sc(2)), "act")
                    rd(lambda e: e.reciprocal(out=sc(3), in_=sc(2)))
                    rd(lambda e: e.tensor_tensor(out=tmp3, in0=rt[:, 8:72].rearrange("p (g e) -> p g e", e=8),
                                                 in1=ohg.unsqueeze(2).to_broadcast([128, 8, 8]), op=ALU.mult))
                    rd(lambda e: e.tensor_reduce(out=fsel, in_=tmp3.rearrange("p g e -> p e g"), axis=AX.X, op=ALU.add))
                    rd(lambda e: e.reduce_max(out=sc(4), in_=fsel, axis=AX.X))
                    rd(lambda e: e.tensor_scalar(out=oh1, in0=fsel, scalar1=sc(4), scalar2=None, op0=ALU.is_equal))
                    rd(lambda e: e.scalar_tensor_tensor(out=msk, in0=oh1, scalar=-1e30, in1=fsel, op0=ALU.mult, op1=ALU.add))
                    rd(lambda e: e.reduce_max(out=sc(5), in_=msk, axis=AX.X))
                    rd(lambda e: e.tensor_scalar(out=oh2, in0=msk, scalar1=sc(5), scalar2=None, op0=ALU.is_equal))
                    rd(lambda e: e.tensor_tensor(out=sc(6), in0=sc(5), in1=sc(4), op=ALU.subtract))
                    rd(lambda e: e.activation(out=sc(7), in_=sc(6), func=AF.Exp), "act")
                    rd(lambda e: e.tensor_scalar(out=sc(8), in0=sc(7), scalar1=1.0, scalar2=None, op0=ALU.add))
                    rd(lambda e: e.reciprocal(out=sc(8), in_=sc(8)))
                    rd(lambda e: e.tensor_tensor(out=sc(9), in0=sc(3), in1=sc(8), op=ALU.mult))
                    rd(lambda e: e.tensor_tensor(out=sc(10), in0=sc(9), in1=sc(7), op=ALU.mult))
                    rd(lambda e: e.tensor_tensor(out=A1.rearrange("p (g e) -> p g e", e=8),
                                                 in0=ohg.unsqueeze(2).to_broadcast([128, 8, 8]),
                                                 in1=oh1.unsqueeze(1).to_broadcast([128, 8, 8]), op=ALU.mult))
                    rd(lambda e: e.tensor_tensor(out=A2.rearrange("p (g e) -> p g e", e=8),
                                                 in0=ohg.unsqueeze(2).to_broadcast([128, 8, 8]),
                                                 in1=oh2.unsqueeze(1).to_broadcast([128, 8, 8]), op=ALU.mult))
                    rd(lambda e: e.tensor_tensor(out=Aa, in0=A1, in1=A2, op=ALU.add))
                    T.I("dve", ["rt"], ["Ab"], lambda e: e.tensor_copy(out=Ab[:], in_=Aa))
                    T.I("dve", ["rt"], ["rt2"], lambda e: e.tensor_scalar(out=WA, in0=A1, scalar1=sc(9), scalar2=None, op0=ALU.mult))
                    T.I("dve", ["rt", "rt2"], ["rt2"], lambda e: e.scalar_tensor_tensor(out=WA, in0=A2, scalar=sc(10), in1=WA,
                                                                                        op0=ALU.mult, op1=ALU.add))
                    T.MM(["Ab", "trib"], ["pb3"], [lambda e: e.matmul(out=pb[3][:, 0:64], lhsT=trib[:], rhs=Ab[:], start=True, stop=True)])
                    T.MM(["Ab", "onesb"], ["pb4"], [lambda e: e.matmul(out=pb[4][:, 0:64], lhsT=onesb[:], rhs=Ab[:], start=True, stop=True)])
                    T.I("dve", ["pb3", "carry", "rt2"], ["rt2"], lambda e: e.tensor_tensor(out=pos, in0=pb[3][:, 0:64], in1=carry[:], op=ALU.add))
                    T.I("dve", ["pb4", "carry"], ["carry"], lambda e: e.tensor_tensor(out=carry[:], in0=pb[4][:, 0:64], in1=carry[:], op=ALU.add))

                    def r2(fn):
                        T.I("dve", ["rt", "rt2", "cst"], ["rt2", "rt"], fn)
                    r2(lambda e: e.tensor_scalar(out=valid, in0=pos, scalar1=128.0, scalar2=None, op0=ALU.is_lt))
                    r2(lambda e: e.tensor_tensor(out=rowv, in0=pos, in1=xcol, op=ALU.add))
                    r2(lambda e: e.scalar_tensor_tensor(out=rowv, in0=rowv, scalar=-8192.0, in1=valid, op0=ALU.add, op1=ALU.mult))
                    r2(lambda e: e.tensor_scalar(out=rowv, in0=rowv, scalar1=8192.0, scalar2=None, op0=ALU.add))
                    r2(lambda e: e.tensor_tensor(out=tmpA, in0=A1, in1=rowv, op=ALU.mult))
                    r2(lambda e: e.reduce_sum(out=sc(11), in_=tmpA, axis=AX.X))
                    r2(lambda e: e.tensor_tensor(out=tmpA, in0=A2, in1=rowv, op=ALU.mult))
                    r2(lambda e: e.reduce_sum(out=sc(12), in_=tmpA, axis=AX.X))
                    T.I("dve", ["rt"], ["slots"], lambda e, ti=ti: e.tensor_copy(out=slots_i[:, ti, :], in_=rt[:, 387:389]))
                    s4, s4res = src4r.next()
                    T.I("dve", ["cst"], [s4res], lambda e, ti=ti, s4=s4: e.tensor_copy(
                        out=s4[:, :, 0], in_=tokid[:, ti:ti + 1].to_broadcast([128, 2])))
                    T.I("dve", ["rt"], [s4res], lambda e, s4=s4: e.tensor_copy(out=s4[:, :, 1], in_=rt[:, 385:387]))
                    for k2 in range(2):
                        T.DMA("pool", "scat", [s4res, "slots", "tblz"], ["tbl_%d_%d" % (ti, k2)],
                              lambda e, s4=s4, ti=ti, k2=k2: e.indirect_dma_start(
                                  out=tbl_d[:, :], out_offset=bass.IndirectOffsetOnAxis(ap=slots_i[:, ti, k2:k2 + 1], axis=0),
                                  in_=s4[:, k2, :], in_offset=None, bounds_check=NEXP * 128 - 1, oob_is_err=False))
                tbl = sb(t0, "tbl", [128, NEXP, 4], F32)
                T.DMA("pool", "tbl", ["tbl_%d_%d" % (ti, k2) for ti in range(NT) for k2 in range(2)], ["tbl"],
                      lambda e: e.dma_start(out=tbl[:], in_=tbl_d.rearrange("(x s) c -> s x c", s=128)))
                T.I("dve", ["tbl"], ["idx_all"], lambda e: e.tensor_copy(out=idx_all[:], in_=tbl[:, :, 0]))
                T.I("dve", ["tbl"], ["w_all"], lambda e: e.tensor_copy(out=w_all[:], in_=tbl[:, :, 1]))
                T.barrier()

            bg(3 * NEXP)
            ds_bg = T.dsem["bg"]
            T.wr["ebf_all"] = ("d_bg", ds_bg[0], ds_bg[1])
            with contextlib.ExitStack() as t0:
                wgr = Ring(t0, "wg", 2, [128, 16, DE], BF16)
                wur = Ring(t0, "wu", 2, [128, 16, DE], BF16)
                wdr = Ring(t0, "wd", 2, [128, 4, D], BF16)
                hxr = Ring(t0, "hx", 2, [128, D], BF16)
                hxTr = Ring(t0, "hxT", 2, [128, 16, 128], BF16)
                sgr = Ring(t0, "sgx", 2, [128, DE], F32)
                ar = Ring(t0, "ax", 2, [128, DE], BF16)
                aTr = Ring(t0, "aTx", 2, [128, 4, 128], BF16)
                yr = Ring(t0, "yx", 2, [128, D], F32)
                zt = sb(t0, "zt", [128, D], F32)
                T.I("dve", [], ["zt"], lambda e: e.memset(zt[:], 0.0))
                T.DMA("sp", "zt", ["zt"], ["y_d"], lambda e: e.dma_start(out=y_d[NEXP * 128:NEXP * 128 + 128, :], in_=zt[:]))
                for x in range(NEXP):
                    wg, rg = wgr.next()
                    wu, ru = wur.next()
                    wd, rdn = wdr.next()
                    T.DMA("pool", rg, ["ebf_all"], [rg], lambda e, wg=wg, x=x: e.dma_start(
                        out=wg[:], in_=eg_bf[x, :, :].rearrange("p (kc n) -> p kc n", kc=16)))
                    T.DMA("pool", ru, ["ebf_all"], [ru], lambda e, wu=wu, x=x: e.dma_start(
                        out=wu[:], in_=eu_bf[x, :, :].rearrange("p (kc n) -> p kc n", kc=16)))
                    T.DMA("pool", rdn, ["ebf_all"], [rdn], lambda e, wd=wd, x=x: e.dma_start(
                        out=wd[:], in_=ed_bf[x, :, :].rearrange("p (kc n) -> p kc n", kc=4)))
                    hx, rhx = hxr.next()
                    T.DMA("pool", rhx, ["idx_all", "h3_d"], [rhx], lambda e, hx=hx, x=x: e.indirect_dma_start(
                        out=hx[:], out_offset=None, in_=h3_d[:, :],
                        in_offset=bass.IndirectOffsetOnAxis(ap=idx_all[:, x:x + 1], axis=0)))
                    hxT, rhxT = hxTr.next()
                    transpose_to_fm(hx, rhx, lambda half, hxT=hxT: hxT[:, half * 8:(half + 1) * 8, :], rhxT)
                    mm_acc(0, lambda kc: hxT[:, kc, :], lambda kc: wg[:, kc, :], 16, [rhxT, rg])
                    mm_acc(1, lambda kc: hxT[:, kc, :], lambda kc: wu[:, kc, :], 16, [rhxT, ru])
                    sg, rsg = sgr.next()
                    at, rat = ar.next()
                    T.I("act", ["pb0"], [rsg], lambda e, sg=sg: e.activation(out=sg[:], in_=pb[0][:, :], func=AF.Silu))
                    T.I("dve", ["pb1", rsg], [rat], lambda e, sg=sg, at=at: e.tensor_tensor(out=at[:], in0=pb[1][:, :], in1=sg[:], op=ALU.mult))
                    aT, raT = aTr.next()
                    T.MM([rat, "identb"], ["tp0"], [lambda e, j=j, at=at: e.transpose(
                        out=tp[0][:, j * 128:(j + 1) * 128], in_=at[:, j * 128:(j + 1) * 128], identity=identb[:]) for j in range(4)])
                    T.I("act", ["tp0"], [raT], lambda e, aT=aT: e.copy(out=aT[:], in_=tp[0][:, 0:512].rearrange("p (j c) -> p j c", c=128)))
                    yt, ry = yr.next()
                    for nb in range(4):
                        bank = 2 + nb % 3
                        mm_acc(bank, lambda kc: aT[:, kc, :], lambda kc: wd[:, kc, nb * 512:(nb + 1) * 512], 4, [raT, rdn])
                        T.I("dve" if nb % 2 == 0 else "act", ["pb%d" % bank, "w_all"], [ry],
                            (lambda e, yt=yt, nb=nb, bank=bank, x=x: e.tensor_scalar(
                                out=yt[:, nb * 512:(nb + 1) * 512], in0=pb[bank][:, :], scalar1=w_all[:, x:x + 1], scalar2=None,
                                op0=ALU.mult)) if nb % 2 == 0 else
                            (lambda e, yt=yt, nb=nb, bank=bank, x=x: e.activation(
                                out=yt[:, nb * 512:(nb + 1) * 512], in_=pb[bank][:, :], func=AF.Copy, scale=w_all[:, x:x + 1])))
                    T.DMA("sp", ry, [ry], ["y_d"], lambda e, yt=yt, x=x: e.dma_start(out=y_d[x * 128:(x + 1) * 128, :], in_=yt[:]))
                T.barrier()

            with contextlib.ExitStack() as t0:
                xr = Ring(t0, "xj", 2, [128, D], F32)
                g1r = Ring(t0, "g1j", 2, [128, D], F32)
                g2r = Ring(t0, "g2j", 2, [128, D], F32)
                orr = Ring(t0, "oj", 2, [128, D], F32)
                junk = sb(t0, "junkj", [128, D], BF16)
                load_gain(4)
                for ti in range(NT):
                    ts_ = slice(ti * 128, (ti + 1) * 128)
                    xt, rx = xr.next()
                    g1, r1 = g1r.next()
                    g2, r2_ = g2r.next()
                    ot, ro = orr.next()
                    T.DMA("sp", rx, [], [rx], lambda e, xt=xt, ts_=ts_: e.dma_start(out=xt[:], in_=x2_d[ts_, :]))
                    T.DMA("pool", r1, ["slots", "y_d"], [r1], lambda e, g1=g1, ti=ti: e.indirect_dma_start(
                        out=g1[:], out_offset=None, in_=y_d[:, :],
                        in_offset=bass.IndirectOffsetOnAxis(ap=slots_i[:, ti, 0:1], axis=0)))
                    T.DMA("pool", r2_, ["slots", "y_d"], [r2_], lambda e, g2=g2, ti=ti: e.indirect_dma_start(
                        out=g2[:], out_offset=None, in_=y_d[:, :],
                        in_offset=bass.IndirectOffsetOnAxis(ap=slots_i[:, ti, 1:2], axis=0)))
                    T.I("dve", [rx, r1], [rx], lambda e, xt=xt, g1=g1: e.tensor_tensor(out=xt[:], in0=xt[:], in1=g1[:], op=ALU.add))
                    T.I("dve", [rx, r2_], [rx], lambda e, xt=xt, g2=g2: e.tensor_tensor(out=xt[:], in0=xt[:], in1=g2[:], op=ALU.add))
                    rms_tile(xt[:], rx, ot[:], ro, junk[:], "junk")
                    T.DMA("sp", ro, [ro], ["out"], lambda e, ot=ot, ts_=ts_: e.dma_start(out=out_d[ts_, :], in_=ot[:]))
                T.barrier()
            phx.close()

        if stop is not None:
            if stop in ("E", "F", "G"):
                for name, src in (("x1", x1_d), ("x2", x2_d)):
                    dd = nc.dram_tensor("dbg_" + name, [TC, D], F32, kind="ExternalOutput").ap()
                    T.DMA("sp", "dbg", [], [], lambda e, dd=dd, src=src: e.dma_start(out=dd[:, :], in_=src[:, :]))
            for name, (tile, shape, dt) in dbg.items():
                dd = nc.dram_tensor("dbg_" + name, list(shape), dt, kind="ExternalOutput").ap()
                T.DMA("sp", "dbg", [name], [], lambda e, dd=dd, tile=tile: e.dma_start(out=dd, in_=tile[:]))
            T.barrier()
            pu.close()
            return nc

        T.barrier()
    return nc


def host_layout(inp):
    f = np.float32
    g = lambda k: np.asarray(inp[k])
    shared = {}
    shared["w_in"] = np.ascontiguousarray(g("w_in")[0])
    shared["w_glu"] = np.ascontiguousarray(g("s5_w_glu")[0])
    shared["w_cwo"] = np.ascontiguousarray(g("conv_w_out")[0])
    shared["w_out"] = np.ascontiguousarray(g("w_out")[0])
    shared["w_q"] = np.ascontiguousarray(g("xattn_wq")[0])
    shared["w_k"] = np.ascontiguousarray(g("xattn_wk")[0])
    shared["w_v"] = np.ascontiguousarray(g("xattn_wv")[0])
    shared["w_o"] = np.ascontiguousarray(g("xattn_wo")[0])
    gn = np.stack([g("norm_mix_g")[0], g("norm_xattn_g")[0], g("norm_mem_g")[0], g("norm_moe_g")[0],
                   g("norm_final_g")], 0)
    shared["gains"] = np.ascontiguousarray(np.broadcast_to(gn[:, None, :], (5, 128, D))).astype(f)
    def l128(a):
        return np.ascontiguousarray(a.reshape(32, 2, 64).transpose(1, 2, 0).reshape(128, 32))
    lre, lim = g("s5_lambda_re")[0], g("s5_lambda_im")[0]
    ldt = np.broadcast_to(g("s5_log_dt")[0][:, None], (64, 64))
    shared["lam3"] = np.ascontiguousarray(np.stack([l128(lre), l128(lim), l128(ldt)], 1)).astype(f)

    def expand(a):
        o = np.zeros((2, 64, 32, 2, 16), f)
        ar = a.reshape(32, 2, 64, 16)
        for par in range(2):
            o[par, :, :, par, :] = ar[:, par].transpose(1, 0, 2)
        return o.reshape(128, 32, 32)
    bre, bim = g("s5_b_re")[0], g("s5_b_im")[0]
    cre = g("s5_c_re")[0].transpose(0, 2, 1)
    cim = g("s5_c_im")[0].transpose(0, 2, 1)
    shared["bc4"] = np.ascontiguousarray(np.stack([expand(bre), expand(bim), expand(cre), expand(cim)], 1))
    chp = np.zeros((128, 8, 36), f)
    cm = lambda v: v.reshape(8, 128).T
    chp[:, :, 0] = cm(g("s5_d")[0])
    chp[:, :, 1] = cm(g("conv_dw_b")[0])
    chp[:, :, 2] = cm(g("conv_ln_g")[0])
    chp[:, :, 3] = cm(g("conv_ln_b")[0])
    chp[:, :, 4:35] = g("conv_dw_w")[0].T.reshape(8, 128, 31).transpose(1, 0, 2)
    shared["chp"] = chp
    wr = np.concatenate([g("router_w_group")[0], g("router_w_expert")[0].transpose(1, 0, 2).reshape(D, 64)], 1)
    shared["wr"] = np.ascontiguousarray(wr).astype(f)
    br = np.concatenate([g("router_b_group")[0], g("router_b_expert")[0].reshape(64)])
    shared["brep"] = np.ascontiguousarray(np.broadcast_to(br[None, :], (128, 72))).astype(f)
    shared["e_g"] = np.ascontiguousarray(g("exp_w_gate")[0])
    shared["e_u"] = np.ascontiguousarray(g("exp_w_up")[0])
    shared["e_d"] = np.ascontiguousarray(g("exp_w_down")[0])
    cst = np.zeros((128, 6, 128), f)
    cst[:, 0, :] = np.eye(128)
    cst[:, 1, :] = np.triu(np.ones((128, 128)), 1)
    cst[:, 2, :] = np.arange(128)[None, :]
    cst[:, 3, :16] = np.arange(16)[None, :] * 128 + np.arange(128)[:, None]
    cst[:, 4, :] = np.kron(np.eye(4), np.ones((32, 32)))
    cst[:, 5, :64] = np.arange(64)[None, :] * 128
    shared["cst"] = cst
    x, mem = g("x"), g("mem")
    per = []
    for c in range(NCORES):
        b, q = c // 4, c % 4
        xmc = np.ascontiguousarray(x[b, q * TC:(q + 1) * TC])
        xhc = np.zeros((HIST, D), f)
        if q > 0:
            xhc[HIST - q * TC:] = x[b, :q * TC]
        per.append({"xm": xmc, "xh": xhc, "memb": np.ascontiguousarray(mem[b])})
    return shared, per


def kernel(**inputs):
    shared, per = host_layout(inputs)
    nc = build_nc()
    in_maps = [dict(shared, **p) for p in per]
    res = run_bass_kernel_spmd(nc, in_maps, core_ids=list(range(NCORES)))
    out = np.zeros((2, 8192, D), np.float32)
    for c in range(NCORES):
        b, q = c // 4, c % 4
        out[b, q * TC:(q + 1) * TC] = res.results[c]["out"]
    return out
```
